# Optimizing a Trainium2 kernel written in Bass

```python
import math
import jax
import jax.numpy as jnp
from jax import lax
import numpy as np

D_MODEL = 1024
BATCH = 4
SEQ = 4096
DEPTH = 1

EPS = 1e-5
SSD_EXPAND = 2
D_SSD = SSD_EXPAND * D_MODEL
SSD_HEAD_DIM = 64
SSD_HEADS = D_SSD // SSD_HEAD_DIM
SSD_GROUPS = 4
SSD_STATE = 128
SSD_CONV = 4
SSD_CHUNK = 128
D_CONV_IN = D_SSD + 2 * SSD_GROUPS * SSD_STATE
POOL_WINDOWS = (2, 4, 8, 16)
N_POOL_GROUPS = len(POOL_WINDOWS)
D_POOL = D_MODEL
POOL_GROUP_DIM = D_POOL // N_POOL_GROUPS
N_BRANCHES = 2
IN_SPLITS = (D_SSD, D_CONV_IN, SSD_HEADS, D_POOL, N_BRANCHES * D_MODEL)
D_IN_PROJ = sum(IN_SPLITS)
N_EXPERTS = 32
TOP_K = 4
D_EXPERT = D_MODEL
SWIGLU_LIMIT = 7.0
SWIGLU_ALPHA = 1.702
MOE_BLOCK = 128

kernel_name = 'hybrid_ssd_pool_moe_adaln_block'


def rms_norm(x, w):
    xf = x.astype(jnp.float32)
    y = xf * lax.rsqrt(jnp.mean(xf * xf, axis=-1, keepdims=True) + EPS)
    return (y * w.astype(jnp.float32)).astype(x.dtype)


def causal_depthwise_conv(u, w, b):
    k, ch = w.shape
    out = lax.conv_general_dilated(u, w[:, None, :], window_strides=(1,), padding=[(k - 1, 0)],
                                   dimension_numbers=('NWC', 'WIO', 'NWC'), feature_group_count=ch)
    return out + b


def ssd_chunked(xh, dt, A, Bm, Cm):
    f32 = jnp.float32
    b, s = xh.shape[0], xh.shape[1]
    nc, L = s // SSD_CHUNK, SSD_CHUNK
    G, E, P, N = SSD_GROUPS, SSD_HEADS // SSD_GROUPS, SSD_HEAD_DIM, SSD_STATE
    X = (xh.astype(f32) * dt[..., None]).reshape(b, nc, L, G, E, P)
    a = jnp.transpose((dt * A).reshape(b, nc, L, G, E), (0, 1, 3, 4, 2))
    Bc = Bm.astype(f32).reshape(b, nc, L, G, N)
    Cc = Cm.astype(f32).reshape(b, nc, L, G, N)
    a_cum = jnp.cumsum(a, axis=-1)
    causal = jnp.tril(jnp.ones((L, L), dtype=bool))
    seg = a_cum[..., :, None] - a_cum[..., None, :]
    decay = jnp.exp(jnp.where(causal, seg, -jnp.inf))
    CB = jnp.einsum('bclgn,bcsgn->bcgls', Cc, Bc)
    y_diag = jnp.einsum('bcgls,bcgels,bcsgep->bclgep', CB, decay, X)
    decay_to_end = jnp.exp(a_cum[..., -1:] - a_cum)
    states = jnp.einsum('bclgn,bcgel,bclgep->bcgepn', Bc, decay_to_end, X)
    chunk_decay = jnp.exp(a_cum[..., -1])

    def step(h, inp):
        st, dec = inp
        return h * dec[..., None, None] + st, h

    h0 = jnp.zeros((b, G, E, P, N), f32)
    _, prev = lax.scan(step, h0, (jnp.moveaxis(states, 1, 0), jnp.moveaxis(chunk_decay, 1, 0)))
    prev = jnp.moveaxis(prev, 0, 1)
    y_off = jnp.einsum('bclgn,bcgepn,bcgel->bclgep', Cc, prev, jnp.exp(a_cum))
    return (y_diag + y_off).reshape(b, s, SSD_HEADS, P)


def multiscale_causal_pool(u):
    f32 = jnp.float32
    b, s, ch = u.shape
    uf = u.astype(f32)
    cs = jnp.concatenate([jnp.zeros((b, 1, ch), f32), jnp.cumsum(uf, axis=1)], axis=1)
    t = jnp.arange(s)
    outs = []
    for gi, w in enumerate(POOL_WINDOWS):
        lo, hi = gi * POOL_GROUP_DIM, (gi + 1) * POOL_GROUP_DIM
        csg = cs[:, :, lo:hi]
        upper = csg[:, 1:]
        lower = jnp.pad(csg[:, :s + 1 - w], ((0, 0), (w - 1, 0), (0, 0)))
        cnt = jnp.minimum(t + 1, w).astype(f32)[None, :, None]
        outs.append((upper - lower) / cnt - uf[:, :, lo:hi])
    return jnp.concatenate(outs, axis=-1).astype(u.dtype)


def hybrid_mixer(h, w_in, conv_w, conv_b, dt_bias, a_log, d_skip, ssd_norm_w, w_ssd_out,
                 w_pool, pool_scale, w_pool_out, w_out):
    f32 = jnp.float32
    b, s, _ = h.shape
    proj = h @ w_in
    z, xbc, dt_raw, u_pool, gates = jnp.split(proj, list(np.cumsum(IN_SPLITS)[:-1]), axis=-1)
    xbc = jax.nn.silu(causal_depthwise_conv(xbc, conv_w, conv_b))
    xs, Bm, Cm = jnp.split(xbc, [D_SSD, D_SSD + SSD_GROUPS * SSD_STATE], axis=-1)
    dt = jax.nn.softplus(dt_raw.astype(f32) + dt_bias.astype(f32))
    A = -jnp.exp(a_log.astype(f32))
    xh = xs.reshape(b, s, SSD_HEADS, SSD_HEAD_DIM)
    y = ssd_chunked(xh, dt, A, Bm.reshape(b, s, SSD_GROUPS, SSD_STATE),
                    Cm.reshape(b, s, SSD_GROUPS, SSD_STATE))
    y = y + d_skip.astype(f32)[:, None] * xh.astype(f32)
    y = y.reshape(b, s, D_SSD) * jax.nn.silu(z.astype(f32))
    yg = y.reshape(b, s, SSD_GROUPS, D_SSD // SSD_GROUPS)
    yg = yg * lax.rsqrt(jnp.mean(yg * yg, axis=-1, keepdims=True) + EPS)
    y = (yg.reshape(b, s, D_SSD) * ssd_norm_w.astype(f32)).astype(h.dtype)
    y_ssd = y @ w_ssd_out
    p = multiscale_causal_pool(u_pool).reshape(b, s, N_POOL_GROUPS, POOL_GROUP_DIM)
    p = jnp.einsum('bsgc,gcd->bsgd', p, w_pool).reshape(b, s, D_POOL) * pool_scale
    y_pool = p @ w_pool_out
    g = jax.nn.sigmoid(gates.astype(f32)).astype(h.dtype)
    g_ssd, g_pool = jnp.split(g, N_BRANCHES, axis=-1)
    return (g_ssd * y_ssd + g_pool * y_pool) @ w_out


def moe_ffn(h, w_router, b_router, w_gu, b_gu, w_down, b_down):
    f32 = jnp.float32
    b, s, d = h.shape
    T = b * s
    xt = h.reshape(T, d)
    logits = xt.astype(f32) @ w_router.astype(f32) + b_router.astype(f32)
    top_val, top_idx = lax.top_k(logits, TOP_K)
    gate = jax.nn.softmax(top_val, axis=-1)
    e_flat = top_idx.reshape(-1).astype(jnp.int32)
    tok_flat = jnp.arange(T * TOP_K, dtype=jnp.int32) // TOP_K
    g_flat = gate.reshape(-1)
    order = jnp.argsort(e_flat)
    e_sorted = e_flat[order]
    counts = jnp.bincount(e_flat, length=N_EXPERTS).astype(jnp.int32)
    starts = jnp.cumsum(counts) - counts
    pad_counts = (counts + MOE_BLOCK - 1) // MOE_BLOCK * MOE_BLOCK
    pad_ends = jnp.cumsum(pad_counts)
    pad_starts = pad_ends - pad_counts
    dest = pad_starts[e_sorted] + (jnp.arange(T * TOP_K, dtype=jnp.int32) - starts[e_sorted])
    n_blocks = (T * TOP_K + MOE_BLOCK - 1) // MOE_BLOCK + N_EXPERTS
    n_rows = n_blocks * MOE_BLOCK
    row_tok = jnp.full((n_rows,), T, jnp.int32).at[dest].set(tok_flat[order])
    row_gate = jnp.zeros((n_rows,), f32).at[dest].set(g_flat[order])
    block_exp = jnp.minimum(jnp.searchsorted(pad_ends, jnp.arange(n_blocks, dtype=jnp.int32) * MOE_BLOCK,
                                             side='right'), N_EXPERTS - 1)
    x_pad = jnp.concatenate([xt, jnp.zeros((1, d), xt.dtype)], axis=0)
    xb = x_pad[row_tok].reshape(n_blocks, MOE_BLOCK, d)

    def expert_block(args):
        xblk, e = args
        gu = xblk @ w_gu[e] + b_gu[e]
        glu, lin = jnp.split(gu, 2, axis=-1)
        glu = jnp.minimum(glu, SWIGLU_LIMIT)
        lin = jnp.clip(lin, -SWIGLU_LIMIT, SWIGLU_LIMIT)
        act = glu * jax.nn.sigmoid(SWIGLU_ALPHA * glu) * (lin + 1.0)
        return act @ w_down[e] + b_down[e]

    yb = lax.map(expert_block, (xb, block_exp)).reshape(n_rows, d)
    y = jax.ops.segment_sum(yb.astype(f32) * row_gate[:, None], row_tok, num_segments=T + 1)[:T]
    return y.reshape(b, s, d).astype(h.dtype)


def setup_inputs(seed: int = 0) -> dict:
    key = jax.random.key(seed)
    ks = jax.random.split(key, 28)
    f32 = jnp.float32
    L = DEPTH

    def nrm(k, shape, scale):
        return jax.random.normal(k, shape, f32) * scale

    dt0 = jnp.exp(jax.random.uniform(ks[8], (L, SSD_HEADS), f32, minval=math.log(1e-3), maxval=math.log(1e-1)))
    return {
        'x': nrm(ks[0], (BATCH, SEQ, D_MODEL), 1.0),
        'c': nrm(ks[1], (BATCH, D_MODEL), 1.0),
        'w_ada': nrm(ks[2], (L, D_MODEL, 6 * D_MODEL), 0.5 * D_MODEL ** -0.5),
        'b_ada': nrm(ks[3], (L, 6 * D_MODEL), 0.02),
        'norm_mix_w': 1.0 + nrm(ks[4], (L, D_MODEL), 0.02),
        'w_in': nrm(ks[5], (L, D_MODEL, D_IN_PROJ), D_MODEL ** -0.5),
        'conv_w': nrm(ks[6], (L, SSD_CONV, D_CONV_IN), SSD_CONV ** -0.5),
        'conv_b': nrm(ks[7], (L, D_CONV_IN), 0.02),
        'dt_bias': dt0 + jnp.log(-jnp.expm1(-dt0)),
        'a_log': jnp.log(jax.random.uniform(ks[9], (L, SSD_HEADS), f32, minval=1.0, maxval=16.0)),
        'd_skip': 1.0 + nrm(ks[10], (L, SSD_HEADS), 0.1),
        'ssd_norm_w': 1.0 + nrm(ks[11], (L, D_SSD), 0.02),
        'w_ssd_out': nrm(ks[12], (L, D_SSD, D_MODEL), D_SSD ** -0.5),
        'w_pool': nrm(ks[13], (L, N_POOL_GROUPS, POOL_GROUP_DIM, POOL_GROUP_DIM), POOL_GROUP_DIM ** -0.5),
        'pool_scale': 1.0 + nrm(ks[14], (L, D_POOL), 0.1),
        'w_pool_out': nrm(ks[15], (L, D_POOL, D_MODEL), D_POOL ** -0.5),
        'w_out': nrm(ks[16], (L, D_MODEL, D_MODEL), D_MODEL ** -0.5),
        'norm_ffn_w': 1.0 + nrm(ks[17], (L, D_MODEL), 0.02),
        'w_router': nrm(ks[18], (L, D_MODEL, N_EXPERTS), D_MODEL ** -0.5),
        'b_router': nrm(ks[19], (L, N_EXPERTS), 0.01),
        'w_gu': nrm(ks[20], (L, N_EXPERTS, D_MODEL, 2 * D_EXPERT), D_MODEL ** -0.5),
        'b_gu': nrm(ks[21], (L, N_EXPERTS, 2 * D_EXPERT), 0.02),
        'w_down': nrm(ks[22], (L, N_EXPERTS, D_EXPERT, D_MODEL), D_EXPERT ** -0.5),
        'b_down': nrm(ks[23], (L, N_EXPERTS, D_MODEL), 0.02),
        'norm_final_w': 1.0 + nrm(ks[24], (D_MODEL,), 0.02),
    }


def reference(x, c, w_ada, b_ada, norm_mix_w, w_in, conv_w, conv_b, dt_bias, a_log, d_skip,
              ssd_norm_w, w_ssd_out, w_pool, pool_scale, w_pool_out, w_out, norm_ffn_w,
              w_router, b_router, w_gu, b_gu, w_down, b_down, norm_final_w):
    c_act = jax.nn.silu(c)
    for l in range(DEPTH):
        mod = (c_act @ w_ada[l] + b_ada[l])[:, None, :]
        sh_m, sc_m, ga_m, sh_f, sc_f, ga_f = jnp.split(mod, 6, axis=-1)
        h = rms_norm(x, norm_mix_w[l]) * (1.0 + sc_m) + sh_m
        x = x + ga_m * hybrid_mixer(h, w_in[l], conv_w[l], conv_b[l], dt_bias[l], a_log[l], d_skip[l],
                                    ssd_norm_w[l], w_ssd_out[l], w_pool[l], pool_scale[l],
                                    w_pool_out[l], w_out[l])
        h = rms_norm(x, norm_ffn_w[l]) * (1.0 + sc_f) + sh_f
        x = x + ga_f * moe_ffn(h, w_router[l], b_router[l], w_gu[l], b_gu[l], w_down[l], b_down[l])
    return rms_norm(x, norm_final_w)
```

```python
import os
import numpy as np
from contextlib import ExitStack
import concourse.bass as bass
import concourse.mybir as mybir
from concourse.bass_utils import run_bass_kernel_spmd

F32 = mybir.dt.float32
BF16 = mybir.dt.bfloat16
AF = mybir.ActivationFunctionType
ALU = mybir.AluOpType
AX = mybir.AxisListType

ENGS = ("pe", "act", "dve", "pool", "sp")
EPS = 1e-5
NEG = -30000.0


SEM_EPOCH = 2000


class SemBox:
    __slots__ = ("sem",)

    def __init__(self, sem):
        self.sem = sem


class Buf:
    __slots__ = ("name", "w", "r", "box", "cnt")

    def __init__(self, name):
        self.name = name
        self.w = None
        self.r = []
        self.box = None
        self.cnt = 0


class Prog:
    def __init__(self, nc, stack, dry=False):
        self.nc = nc
        self.stack = stack
        self.dry = dry
        self.ops = {e: [] for e in ENGS}
        self.seq = {e: 0 for e in ENGS}
        self.waited = {e: {} for e in ENGS}
        self.esem = {e: [] for e in ENGS}
        self.nsem = 0
        self.out_tokens = []

    def _esem(self, eng, n):
        e = (n - 1) // SEM_EPOCH
        while len(self.esem[eng]) <= e:
            self.esem[eng].append(self.stack.enter_context(self.nc.semaphore("es_%s_%d" % (eng, len(self.esem[eng])))))
        return self.esem[eng][e], (n - 1) % SEM_EPOCH + 1

    def _sem_of(self, key, v):
        if isinstance(key, str):
            return self._esem(key, v)
        return key.sem, v

    def _waits(self, eng, reads, writes):
        need = {}
        for b in reads:
            if b.w is not None:
                k, v = b.w
                need[k] = max(need.get(k, 0), v)
        for b in writes:
            if b.w is not None:
                k, v = b.w
                need[k] = max(need.get(k, 0), v)
            for (k, v) in b.r:
                need[k] = max(need.get(k, 0), v)
        out = []
        wd = self.waited[eng]
        for k, v in need.items():
            if k == "pe" and eng == "pe":
                continue
            if wd.get(k, 0) >= v:
                continue
            wd[k] = v
            out.append((k, v))
        return out

    def op(self, eng, fn, reads=(), writes=()):
        if self.dry:
            return
        waits = self._waits(eng, reads, writes)
        self.seq[eng] += 1
        tok = (eng, self.seq[eng])
        for b in reads:
            b.r.append(tok)
            if len(b.r) > 64:
                best = {}
                for (k, v) in b.r:
                    best[k] = max(best.get(k, 0), v)
                b.r = list(best.items())
        for b in writes:
            b.w = tok
            b.r = []
        self.ops[eng].append((waits, fn, None))

    def dma(self, eng, pairs, reads=(), writes=(), is_output=False):
        if self.dry:
            return
        if not isinstance(pairs, list):
            pairs = [pairs]
        owner = writes[0] if writes else reads[0]
        if owner.box is None or owner.cnt + 16 * len(pairs) > SEM_EPOCH:
            owner.box = SemBox(self.stack.enter_context(self.nc.semaphore("ds_%d" % self.nsem)))
            owner.cnt = 0
            self.nsem += 1
        waits = self._waits(eng, reads, writes)
        owner.cnt += 16 * len(pairs)
        box = owner.box
        tok = (box, owner.cnt)
        for b in reads:
            b.r.append(tok)
        for b in writes:
            b.w = tok
            b.r = []
        for i, (o, a) in enumerate(pairs):
            self.ops[eng].append((waits if i == 0 else [], None, (o, a, box)))
        if is_output:
            self.out_tokens.append(tok)

    def emit(self, block):
        def run(eng, h):
            n = 0
            for waits, fn, d in self.ops[eng]:
                for k, v in waits:
                    sem, val = self._sem_of(k, v)
                    h.wait_ge(sem, val)
                if fn is not None:
                    n += 1
                    sem, _ = self._esem(eng, n)
                    fn(h).then_inc(sem, 1)
                else:
                    o, a, box = d
                    h.dma_start(out=o, in_=a).then_inc(box.sem, 16)
            if eng == "sp":
                for k, v in self.out_tokens:
                    sem, val = self._sem_of(k, v)
                    h.wait_ge(sem, val)

        @block.tensor
        def _(h):
            run("pe", h)

        @block.scalar
        def _(h):
            run("act", h)

        @block.vector
        def _(h):
            run("dve", h)

        @block.gpsimd
        def _(h):
            run("pool", h)

        @block.sync
        def _(h):
            run("sp", h)


T_CORE = 2048
D = 1024
NSC = 4
RING = 3
N_EXP = 32
DEBUG_X1 = False


_LAST = {}


class _Stop(Exception):
    pass


class QuietStack(ExitStack):
    def __exit__(self, et, ev, tb):
        super().__exit__(None, None, None)
        return False


def build_nc(n_exp=N_EXP, debug=False, stop=0, n_w=32):
    nc = bass.Bass("TRN2", target_bir_lowering=False)

    def din(name, shape, dt=F32):
        return nc.dram_tensor(name, list(shape), dt, kind="ExternalInput").ap()

    x_own = din("x_own", [T_CORE, D])
    x_pre = din("x_pre", [T_CORE, D])
    flag_d = din("flag", [128, 1])
    invcnt_d = din("invcnt", [128, 4, 512])
    c_col_d = din("c_col", [128, 8])
    w_ada = din("w_ada", [D, 6 * D])
    bada_col_d = din("bada_col", [128, 48])
    bada_row_d = din("bada_row", [128, 2048])
    nmw_col_d = din("nmw_col", [128, 8])
    nfw_col_d = din("nfw_col", [128, 8])
    nfin_row_d = din("nfin_row", [128, D])
    w_in = din("w_in", [D, 8224])
    convw_col_d = din("convw_col", [128, 24, 4])
    convb_col_d = din("convb_col", [128, 24])
    dtb_row_d = din("dtb_row", [128, 32])
    alog_row_d = din("alog_row", [128, 32])
    dskip_row_d = din("dskip_row", [128, 32])
    ssdnw_col_d = din("ssdnw_col", [128, 16])
    w_ssd_out = din("w_ssd_out", [2048, D])
    w_pool = din("w_pool", [1024, 256])
    pscale_col_d = din("pscale_col", [128, 8])
    w_pool_out = din("w_pool_out", [D, D])
    w_out = din("w_out", [D, D])
    w_router_d = din("w_router", [D, 32])
    brouter_row_d = din("brouter_row", [128, 32])
    w_gu = din("w_gu", [n_w, D, 2048])
    bgu_col_d = din("bgu_col", [128, 32, 16])
    w_down = din("w_down", [n_w, D, D])
    b_down_d = din("b_down", [128, D])
    ident_d = din("ident", [128, 128])
    utri_d = din("utri", [128, 128])
    negm4_d = din("negm4", [128, 512])
    out_d = nc.dram_tensor("out", [T_CORE, D], F32, kind="ExternalOutput").ap()
    x1_d = nc.dram_tensor("x1_scratch", [T_CORE, D], F32, kind=("ExternalOutput" if debug else "Internal")).ap()

    with ExitStack() as st:
        def sb(name, shape, dt=F32):
            t = st.enter_context(nc.sbuf_tensor("s_" + name, list(shape), dt))
            return t, Buf(name)

        def psum(name, shape, dt=F32):
            t = st.enter_context(nc.psum_tensor(name, list(shape), dt))
            return t, Buf(name)

        ident, b_ident = sb("ident", [128, 128])
        identb, b_identb = sb("identb", [128, 128], BF16)
        utri, b_utri = sb("utri", [128, 128])
        ones32, b_ones32 = sb("ones32", [128, 128])
        negm4, b_negm4 = sb("negm4", [128, 512])
        flag, b_flag = sb("flag", [128, 1])
        c_col, b_ccol = sb("c_col", [128, 8])
        c_act, b_cact = sb("c_act", [128, 8], BF16)
        cbc, b_cbc = sb("cbc", [128, 8, 128], BF16)
        bada_col, b_badac = sb("bada_col", [128, 48])
        modcol, b_modcol = sb("modcol", [128, 48])
        garow, b_garow = sb("garow", [128, 2048])
        nmw_col, b_nmw = sb("nmw_col", [128, 8])
        nfw_col, b_nfw = sb("nfw_col", [128, 8])
        am_col, b_am = sb("am_col", [128, 8])
        af_col, b_af = sb("af_col", [128, 8])
        convw, b_convw = sb("convw", [128, 24, 4])
        convb, b_convb = sb("convb", [128, 24])
        dtb_row, b_dtb = sb("dtb_row", [128, 32])
        A_row, b_A = sb("A_row", [128, 32])
        dskip_row, b_dskip = sb("dskip_row", [128, 32])
        pscale, b_pscale = sb("pscale", [128, 8])
        PARAMS = [b_ident, b_identb, b_utri, b_ones32, b_negm4, b_flag, b_cact, b_cbc, b_modcol, b_garow,
                  b_am, b_af, b_convw, b_convb, b_dtb, b_A, b_dskip, b_pscale]

        fbanks = [psum("pf%d" % i, [128, 512]) for i in range(6)]
        tbanks = [psum("pt%d" % i, [128, 1024], BF16) for i in range(2)]
        rr = {"f": 0, "t": 0}

        def fbank():
            r = fbanks[rr["f"] % 6]
            rr["f"] += 1
            return r

        def tbank():
            r = tbanks[rr["t"] % 2]
            rr["t"] += 1
            return r

        ring = [sb("ring%d" % i, [128, 8, 512], BF16) for i in range(RING)]

        plan = []
        state = {"P": None, "issued": 0, "taken": 0, "run": 0}

        def piece(W2d, k0, c0, w):
            P = state["P"]
            if P.dry:
                plan.append((W2d, k0, c0, w))
                return ring[0][0], ring[0][1]
            i = state["taken"]
            assert plan[i][1:] == (k0, c0, w), (i, plan[i][1:], (k0, c0, w))
            while state["issued"] < min(len(plan), i + RING - 1):
                j = state["issued"]
                Wj, kj, cj, wj = plan[j]
                t, b = ring[j % RING]
                src = Wj[kj * 128:(kj + 8) * 128, cj:cj + wj].rearrange("(k p) c -> p k c", p=128)
                P.dma("pool", (t[:, :, 0:wj], src), writes=[b])
                state["issued"] += 1
            state["taken"] += 1
            return ring[i % RING]

        def program(P):
            state["P"] = P
            state["run"] += 1
            state["issued"] = 0
            state["taken"] = 0
            rr["f"] = 0
            rr["t"] = 0

            def ck(n):
                if stop == n:
                    raise _Stop()

            def PE(fn, r, w): P.op("pe", fn, r, w)
            def ACT(fn, r, w): P.op("act", fn, r, w)
            def DVE(fn, r, w): P.op("dve", fn, r, w)
            def LD(dst, src, b): P.dma("sp", (dst, src), writes=[b])

            LD(ident[:], ident_d, b_ident)
            LD(utri[:], utri_d, b_utri)
            LD(negm4[:], negm4_d, b_negm4)
            LD(flag[:], flag_d, b_flag)
            LD(c_col[:], c_col_d, b_ccol)
            LD(bada_col[:], bada_col_d, b_badac)
            LD(garow[:], bada_row_d, b_garow)
            LD(nmw_col[:], nmw_col_d, b_nmw)
            LD(nfw_col[:], nfw_col_d, b_nfw)
            LD(convw[:], convw_col_d, b_convw)
            LD(convb[:], convb_col_d, b_convb)
            LD(dtb_row[:], dtb_row_d, b_dtb)
            LD(A_row[:], alog_row_d, b_A)
            LD(dskip_row[:], dskip_row_d, b_dskip)
            LD(pscale[:], pscale_col_d, b_pscale)
            DVE(lambda e: e.tensor_copy(identb[:], ident[:]), [b_ident], [b_identb])
            DVE(lambda e: e.memset(ones32[:], 1.0), [], [b_ones32])
            ACT(lambda e: e.activation(A_row[:], A_row[:], AF.Exp), [b_A], [b_A])
            DVE(lambda e: e.tensor_scalar(A_row[:], A_row[:], -1.0, None, ALU.mult), [b_A], [b_A])
            ACT(lambda e: e.activation(c_act[:], c_col[:], AF.Silu), [b_ccol], [b_cact])
            DVE(lambda e: e.tensor_copy(cbc[:], c_act[:].unsqueeze(2).to_broadcast([128, 8, 128])), [b_cact], [b_cbc])

            for j in range(12):
                pc, bpc = piece(w_ada, 0, j * 512, 512)
                ps, bps = fbank()
                if j in (4, 5, 10, 11):
                    half = {4: 0, 5: 1, 10: 2, 11: 3}[j]
                    for k in range(8):
                        PE(lambda e, k=k, pc=pc, ps=ps: e.matmul(ps[:, 0:512], cbc[:, k, :], pc[:, k, :], start=(k == 0), stop=(k == 7)),
                           [b_cbc, bpc], [bps])
                    DVE(lambda e, ps=ps, half=half: e.tensor_tensor(garow[:, half * 512:(half + 1) * 512], ps[:, 0:512],
                                                                    garow[:, half * 512:(half + 1) * 512], ALU.add), [bps, b_garow], [b_garow])
                else:
                    for mt in range(4):
                        for k in range(8):
                            PE(lambda e, k=k, mt=mt, pc=pc, ps=ps: e.matmul(ps[:, mt:mt + 1], pc[:, k, mt * 128:(mt + 1) * 128], c_act[:, k:k + 1],
                                                                            start=(k == 0), stop=(k == 7)), [b_cact, bpc], [bps])
                    DVE(lambda e, ps=ps, j=j: e.tensor_tensor(modcol[:, j * 4:(j + 1) * 4], ps[:, 0:4], bada_col[:, j * 4:(j + 1) * 4], ALU.add),
                        [bps, b_badac], [b_modcol])
            DVE(lambda e: e.scalar_tensor_tensor(am_col[:], modcol[:, 8:16], 1.0, nmw_col[:], ALU.add, ALU.mult), [b_modcol, b_nmw], [b_am])
            DVE(lambda e: e.scalar_tensor_tensor(af_col[:], modcol[:, 32:40], 1.0, nfw_col[:], ALU.add, ALU.mult), [b_modcol, b_nfw], [b_af])
            ck(1)

            def rms_to_T(xt, bxt, a_col, s_col0, dstT, bdst, col0, scr, bscr, st_, bst, h32=None, bh32=None):
                ACT(lambda e: e.activation(scr[:, 0:D], xt, AF.Square), [bxt], [bscr])
                DVE(lambda e: e.reduce_sum(st_[:, 0:1], scr[:, 0:D], axis=AX.X), [bscr], [bst])
                DVE(lambda e: e.tensor_scalar(st_[:, 1:2], st_[:, 0:1], 1.0 / D, EPS, ALU.mult, ALU.add), [bst], [bst])
                ACT(lambda e: e.activation(st_[:, 3:4], st_[:, 1:2], AF.Sqrt), [bst], [bst])
                DVE(lambda e: e.reciprocal(st_[:, 2:3], st_[:, 3:4]), [bst], [bst])
                ACT(lambda e: e.activation(scr[:, 0:D], xt, AF.Identity, scale=st_[:, 2:3]), [bxt, bst], [bscr])
                for hh in range(2):
                    ps, bps = fbank()
                    for q in range(4):
                        k = hh * 4 + q
                        PE(lambda e, k=k, q=q, ps=ps: e.transpose(ps[:, q * 128:(q + 1) * 128], scr[:, k * 128:(k + 1) * 128], ident[:]),
                           [bscr, b_ident], [bps])
                    for q in range(4):
                        k = hh * 4 + q
                        ACT(lambda e, k=k, q=q, ps=ps: e.activation(dstT[:, k, col0:col0 + 128], ps[:, q * 128:(q + 1) * 128], AF.Identity,
                                                                    bias=modcol[:, s_col0 + k:s_col0 + k + 1], scale=a_col[:, k:k + 1]),
                            [bps, b_modcol, b_am, b_af], [bdst])
                        if h32 is not None:
                            ACT(lambda e, k=k, q=q, ps=ps: e.activation(h32[:, k, :], ps[:, q * 128:(q + 1) * 128], AF.Identity,
                                                                        bias=modcol[:, s_col0 + k:s_col0 + k + 1], scale=a_col[:, k:k + 1]),
                                [bps, b_modcol, b_am, b_af], [bh32])

            with QuietStack() as ms:
                def msb(name, shape, dt=F32):
                    t = ms.enter_context(nc.sbuf_tensor("m%d_" % state["run"] + name, list(shape), dt))
                    return t, Buf(name)

                xt4 = [msb("xt%d" % i, [128, D]) for i in range(2)]
                scr, b_scr = msb("scr", [128, D])
                stt, b_stt = msb("stt", [128, 8])
                hT, b_hT = msb("hT", [128, 8, 512], BF16)
                zs, b_zs = msb("zs", [128, 4, 2048], BF16)
                xc, b_xc = msb("xc", [128, 24, 512], BF16)
                ub = [msb("ub%d" % i, [128, 515]) for i in range(2)]
                acc, b_acc = msb("acc", [128, 512])
                halo, b_halo = msb("halo", [128, 24, 3])
                pb, b_pb = msb("pb", [128, 527])
                ps1, b_ps1 = msb("ps1", [128, 527])
                ps2, b_ps2 = msb("ps2", [128, 527])
                phalo, b_phalo = msb("phalo", [128, 8, 15])
                ivt, b_ivt = msb("ivt", [128, 512])
                big16, b_big = msb("big16", [128, 16, 512], BF16)
                pooledT, b_pooledT = big16[:, 0:8, :], b_big
                pT, b_pT = big16[:, 8:16, :], b_big
                ynT, b_ynT = big16, b_big
                dtt, b_dtt = msb("dtt", [128, 4, 32])
                aa, b_aa = msb("aa", [128, 4, 32])
                gs, b_gs = msb("gs", [128, 8, 512], BF16)
                mergedT, b_mergedT = msb("mergedT", [128, 8, 512], BF16)
                hst, b_hst = msb("hst", [128, 2048])
                hprev, b_hprev = msb("hprev", [128, 2048], BF16)
                ssdnw, b_ssdnw = msb("ssdnw", [128, 16])
                sm, b_sm = msb("sm", [128, 8, 32])
                Xb, b_Xb = msb("Xb", [128, 2048], BF16)
                Xw, b_Xw = msb("Xw", [128, 2048], BF16)
                ynb, b_ynb = Xw, b_Xw
                Bt, b_Bt = msb("Bt", [128, 512], BF16)
                yk, b_yk = msb("yk", [128, 2048])
                cbT, b_cbT = msb("cbT", [128, 128])
                aU, b_aU = msb("aU", [128, 4, 128])
                dec, b_dec = msb("dec", [128, 4, 128])
                MT, b_MT = msb("MT", [128, 4, 128], BF16)
                tmpg, b_tmpg = msb("tmpg", [128, 512])
                ssg, b_ssg = msb("ssg", [128, 8])

                LD(ssdnw[:], ssdnw_col_d, b_ssdnw)
                DVE(lambda e: e.memset(halo[:], 0.0), [], [b_halo])
                DVE(lambda e: e.memset(ps1[:], 0.0), [], [b_ps1])
                DVE(lambda e: e.memset(ps2[:], 0.0), [], [b_ps2])
                DVE(lambda e: e.memset(phalo[:], 0.0), [], [b_phalo])
                DVE(lambda e: e.memset(hst[:], 0.0), [], [b_hst])
                DVE(lambda e: e.memset(hprev[:], 0.0), [], [b_hprev])

                def feat_major_tiles(pc, bpc, n_mt, m0, consumer):
                    for mt in range(n_mt):
                        ps, bps = fbank()
                        for k in range(8):
                            PE(lambda e, k=k, mt=mt, pc=pc, ps=ps: e.matmul(ps[:, 0:512], pc[:, k, mt * 128:(mt + 1) * 128], hT[:, k, :],
                                                                            start=(k == 0), stop=(k == 7)), [bpc, b_hT], [bps])
                        consumer(m0 + mt, ps, bps)

                ubi = {"i": 0}

                def conv_consumer(m, ps, bps):
                    u, bu = ub[ubi["i"] % 2]
                    ubi["i"] += 1
                    ACT(lambda e: e.activation(u[:, 3:515], ps[:, 0:512], AF.Identity), [bps], [bu])
                    DVE(lambda e: e.tensor_copy(u[:, 0:3], halo[:, m, :]), [b_halo], [bu])
                    DVE(lambda e: e.tensor_copy(halo[:, m, :], u[:, 512:515]), [bu], [b_halo])
                    DVE(lambda e: e.tensor_scalar(acc[:], u[:, 0:512], convw[:, m, 0:1], None, ALU.mult), [bu, b_convw], [b_acc])
                    for j in range(1, 4):
                        DVE(lambda e, j=j: e.scalar_tensor_tensor(acc[:], u[:, j:j + 512], convw[:, m, j:j + 1], acc[:], ALU.mult, ALU.add),
                            [bu, b_convw, b_acc], [b_acc])
                    ACT(lambda e: e.activation(xc[:, m, :], acc[:], AF.Silu, bias=convb[:, m:m + 1]), [b_acc, b_convb], [b_xc])

                def make_pool_consumer(first_own):
                    def pool_consumer(m, ps, bps):
                        wi = m // 2
                        w = (2, 4, 8, 16)[wi]
                        ACT(lambda e: e.activation(pb[:, 15:527], ps[:, 0:512], AF.Identity), [bps], [b_pb])
                        DVE(lambda e: e.tensor_copy(pb[:, 0:15], phalo[:, m, :]), [b_phalo], [b_pb])
                        DVE(lambda e: e.tensor_copy(phalo[:, m, :], pb[:, 512:527]), [b_pb], [b_phalo])
                        cur, bcur = pb, b_pb
                        nxt = [(ps1, b_ps1), (ps2, b_ps2)]
                        ni = 0
                        sh = 1
                        while sh < w:
                            nt, bnt = nxt[ni % 2]
                            ni += 1
                            DVE(lambda e, nt=nt, cur=cur, sh=sh: e.tensor_tensor(nt[:, sh:527], cur[:, sh:527], cur[:, 0:527 - sh], ALU.add),
                                [bcur], [bnt])
                            cur, bcur = nt, bnt
                            sh *= 2
                        if first_own:
                            LD(ivt[:], invcnt_d[:, wi, :], b_ivt)
                            DVE(lambda e, cur=cur: e.tensor_tensor(acc[:], cur[:, 15:527], ivt[:], ALU.mult), [bcur, b_ivt], [b_acc])
                            DVE(lambda e: e.tensor_tensor(pooledT[:, m, :], acc[:], pb[:, 15:527], ALU.subtract), [b_acc, b_pb], [b_pooledT])
                        else:
                            DVE(lambda e, cur=cur: e.scalar_tensor_tensor(pooledT[:, m, :], cur[:, 15:527], 1.0 / w, pb[:, 15:527], ALU.mult, ALU.subtract),
                                [bcur, b_pb], [b_pooledT])
                    return pool_consumer

                def ssd_chunk(cq, full):
                    cs = cq * 128
                    ps, bps = fbank()
                    PE(lambda e: e.matmul(ps[:, 0:32], utri[:], aa[:, cq, :], start=True, stop=True), [b_utri, b_aa], [bps])
                    PE(lambda e: e.matmul(ps[:, 32:64], ones32[:], aa[:, cq, :], start=True, stop=True), [b_ones32, b_aa], [bps])
                    DVE(lambda e: e.tensor_copy(sm[:, 0, :], ps[:, 0:32]), [bps], [b_sm])
                    DVE(lambda e: e.tensor_scalar(sm[:, 1, :], ps[:, 0:32], -1.0, None, ALU.mult), [bps], [b_sm])
                    DVE(lambda e: e.tensor_copy(sm[:, 2, :], ps[:, 32:64]), [bps], [b_sm])
                    DVE(lambda e: e.tensor_tensor(sm[:, 6, :], sm[:, 2, :], sm[:, 0, :], ALU.subtract), [b_sm], [b_sm])
                    ACT(lambda e: e.activation(sm[:, 3, :], sm[:, 0, :], AF.Exp), [b_sm], [b_sm])
                    ACT(lambda e: e.activation(sm[:, 4, :], sm[:, 6, :], AF.Exp), [b_sm], [b_sm])
                    ACT(lambda e: e.activation(sm[:, 5, :], sm[:, 2, :], AF.Exp), [b_sm], [b_sm])
                    for hh in range(2):
                        pt, bpt = tbank()
                        for q in range(8):
                            m = hh * 8 + q
                            PE(lambda e, m=m, q=q, pt=pt: e.transpose(pt[:, q * 128:(q + 1) * 128], xc[:, m, cs:cs + 128], identb[:]),
                               [b_xc, b_identb], [bpt])
                        sl = slice(hh * 1024, (hh + 1) * 1024)
                        hsl = slice(hh * 16, (hh + 1) * 16)
                        DVE(lambda e, pt=pt, sl=sl, hsl=hsl: e.tensor_tensor(
                            Xb[:, sl].rearrange("p (h d) -> p h d", h=16), pt[:].rearrange("p (h d) -> p h d", h=16),
                            dtt[:, cq, hsl].unsqueeze(2).to_broadcast([128, 16, 64]), ALU.mult), [bpt, b_dtt], [b_Xb])
                        if full:
                            DVE(lambda e, pt=pt, sl=sl, hsl=hsl: e.tensor_tensor(
                                yk[:, sl].rearrange("p (h d) -> p h d", h=16), pt[:].rearrange("p (h d) -> p h d", h=16),
                                dskip_row[:, hsl].unsqueeze(2).to_broadcast([128, 16, 64]), ALU.mult), [bpt, b_dskip], [b_yk])
                    pt, bpt = tbank()
                    for g in range(4):
                        PE(lambda e, g=g, pt=pt: e.transpose(pt[:, g * 128:(g + 1) * 128], xc[:, 16 + g, cs:cs + 128], identb[:]),
                           [b_xc, b_identb], [bpt])
                    ACT(lambda e, pt=pt: e.activation(Bt[:], pt[:, 0:512], AF.Identity), [bpt], [b_Bt])
                    DVE(lambda e: e.tensor_tensor(Xw[:].rearrange("p (h d) -> p h d", h=32), Xb[:].rearrange("p (h d) -> p h d", h=32),
                                                  sm[:, 4, :].unsqueeze(2).to_broadcast([128, 32, 64]), ALU.mult), [b_Xb, b_sm], [b_Xw])
                    for g in range(4):
                        gsl = slice(g * 512, (g + 1) * 512)
                        if full:
                            pcb, bpcb = fbank()
                            PE(lambda e, pcb=pcb, g=g: e.matmul(pcb[:, 0:128], xc[:, 16 + g, cs:cs + 128], xc[:, 20 + g, cs:cs + 128], start=True, stop=True),
                               [b_xc], [bpcb])
                            ACT(lambda e, pcb=pcb: e.activation(cbT[:], pcb[:, 0:128], AF.Identity), [bpcb], [b_cbT])
                            py, bpy = fbank()
                            for hq in range(2):
                                h0 = g * 8 + hq * 4
                                DVE(lambda e, h0=h0: e.tensor_tensor(aU[:], utri[:].unsqueeze(1).to_broadcast([128, 4, 128]),
                                                                     aa[:, cq, h0:h0 + 4].unsqueeze(2).to_broadcast([128, 4, 128]), ALU.mult),
                                    [b_utri, b_aa], [b_aU])
                                pg, bpg = fbank()
                                PE(lambda e, pg=pg: e.matmul(pg[:, 0:512], ones32[:], aU[:].rearrange("p h l -> p (h l)"), start=True, stop=False),
                                   [b_ones32, b_aU], [bpg])
                                PE(lambda e, pg=pg: e.matmul(pg[:, 0:512], ident[:], negm4[:], start=False, stop=True), [b_ident, b_negm4], [bpg])
                                for j in range(4):
                                    ACT(lambda e, j=j, h0=h0, pg=pg: e.activation(dec[:, j, :], pg[:, j * 128:(j + 1) * 128], AF.Exp,
                                                                                  bias=sm[:, 1, h0 + j:h0 + j + 1]), [bpg, b_sm], [b_dec])
                                DVE(lambda e: e.tensor_tensor(MT[:], dec[:], cbT[:].unsqueeze(1).to_broadcast([128, 4, 128]), ALU.mult),
                                    [b_dec, b_cbT], [b_MT])
                                for j in range(4):
                                    h = h0 + j
                                    c0 = (hq * 4 + j) * 64
                                    PE(lambda e, j=j, h=h, c0=c0, py=py: e.matmul(py[:, c0:c0 + 64], MT[:, j, :], Xb[:, h * 64:(h + 1) * 64],
                                                                                  start=True, stop=True), [b_MT, b_Xb], [bpy])
                            po, bpo = fbank()
                            PE(lambda e, po=po, gsl=gsl, g=g: e.matmul(po[:, 0:512], xc[:, 20 + g, cs:cs + 128], hprev[:, gsl], start=True, stop=True),
                               [b_xc, b_hprev], [bpo])
                            DVE(lambda e, po=po, g=g: e.tensor_tensor(tmpg[:].rearrange("p (h d) -> p h d", h=8), po[:].rearrange("p (h d) -> p h d", h=8),
                                                                 sm[:, 3, g * 8:(g + 1) * 8].unsqueeze(2).to_broadcast([128, 8, 64]), ALU.mult),
                                [bpo, b_sm], [b_tmpg])
                            DVE(lambda e, py=py: e.tensor_tensor(tmpg[:], tmpg[:], py[:, 0:512], ALU.add), [b_tmpg, bpy], [b_tmpg])
                            DVE(lambda e, gsl=gsl: e.tensor_tensor(yk[:, gsl], yk[:, gsl], tmpg[:], ALU.add), [b_yk, b_tmpg], [b_yk])
                        pst, bpst = fbank()
                        PE(lambda e, pst=pst, gsl=gsl, g=g: e.matmul(pst[:, 0:512], Bt[:, g * 128:(g + 1) * 128], Xw[:, gsl], start=True, stop=True),
                           [b_Bt, b_Xw], [bpst])
                        DVE(lambda e, gsl=gsl, g=g: e.tensor_tensor(hst[:, gsl].rearrange("p (h d) -> p h d", h=8), hst[:, gsl].rearrange("p (h d) -> p h d", h=8),
                                                               sm[:, 5, g * 8:(g + 1) * 8].unsqueeze(2).to_broadcast([128, 8, 64]), ALU.mult),
                            [b_hst, b_sm], [b_hst])
                        DVE(lambda e, pst=pst, gsl=gsl: e.tensor_tensor(hst[:, gsl], hst[:, gsl], pst[:, 0:512], ALU.add), [b_hst, bpst], [b_hst])
                        ACT(lambda e, gsl=gsl: e.activation(hprev[:, gsl], hst[:, gsl], AF.Identity), [b_hst], [b_hprev])
                    if not full or os.environ.get('KDBG') == 'nossd':
                        return
                    DVE(lambda e: e.tensor_tensor(yk[:], yk[:], zs[:, cq, :], ALU.mult), [b_yk, b_zs], [b_yk])
                    for hh in range(2):
                        ACT(lambda e, hh=hh: e.activation(scr[:, 0:D], yk[:, hh * 1024:(hh + 1) * 1024], AF.Square), [b_yk], [b_scr])
                        DVE(lambda e, hh=hh: e.reduce_sum(ssg[:, hh * 2:hh * 2 + 2], scr[:, 0:D].rearrange("p (g d) -> p g d", g=2), axis=AX.X),
                            [b_scr], [b_ssg])
                    DVE(lambda e: e.tensor_scalar(ssg[:, 0:4], ssg[:, 0:4], 1.0 / 512, EPS, ALU.mult, ALU.add), [b_ssg], [b_ssg])
                    ACT(lambda e: e.activation(ssg[:, 4:8], ssg[:, 0:4], AF.Sqrt), [b_ssg], [b_ssg])
                    DVE(lambda e: e.reciprocal(ssg[:, 4:8], ssg[:, 4:8]), [b_ssg], [b_ssg])
                    DVE(lambda e: e.tensor_tensor(ynb[:].rearrange("p (g d) -> p g d", g=4), yk[:].rearrange("p (g d) -> p g d", g=4),
                                                  ssg[:, 4:8].unsqueeze(2).to_broadcast([128, 4, 512]), ALU.mult), [b_yk, b_ssg], [b_ynb])
                    for hh in range(2):
                        pt, bpt = tbank()
                        for q in range(8):
                            m = hh * 8 + q
                            PE(lambda e, m=m, q=q, pt=pt: e.transpose(pt[:, q * 128:(q + 1) * 128], ynb[:, m * 128:(m + 1) * 128], identb[:]),
                               [b_ynb, b_identb], [bpt])
                        for q in range(8):
                            m = hh * 8 + q
                            ACT(lambda e, pt=pt, m=m, q=q: e.activation(ynT[:, m, cs:cs + 128], pt[:, q * 128:(q + 1) * 128], AF.Identity,
                                                                        scale=ssdnw[:, m:m + 1]), [bpt, b_ssdnw], [b_ynT])

                sc_list = [("pre", i) for i in range(NSC)] + [("own", i) for i in range(NSC)]
                for (kind, si) in sc_list:
                    full = kind == "own"
                    xsrc = x_own if full else x_pre
                    last_pre = (kind == "pre" and si == NSC - 1)
                    for tt in range(4):
                        xt, bxt = xt4[tt % 2]
                        r0 = si * 512 + tt * 128
                        LD(xt[:], xsrc[r0:r0 + 128, :], bxt)
                        rms_to_T(xt[:], bxt, am_col, 0, hT, b_hT, tt * 128, scr, b_scr, stt, b_stt)
                    ck(2)
                    if full:
                        for zi in range(4):
                            pc, bpc = piece(w_in, 0, zi * 512, 512)
                            for tt in range(4):
                                ps, bps = fbank()
                                for k in range(8):
                                    PE(lambda e, k=k, tt=tt, pc=pc, ps=ps: e.matmul(ps[:, 0:512], hT[:, k, tt * 128:(tt + 1) * 128], pc[:, k, :],
                                                                                    start=(k == 0), stop=(k == 7)), [bpc, b_hT], [bps])
                                ACT(lambda e, tt=tt, zi=zi, ps=ps: e.activation(zs[:, tt, zi * 512:(zi + 1) * 512], ps[:, 0:512], AF.Silu), [bps], [b_zs])
                    for xi in range(4):
                        pc, bpc = piece(w_in, 0, 2048 + xi * 512, 512)
                        feat_major_tiles(pc, bpc, 4, xi * 4, conv_consumer)
                    pc, bpc = piece(w_in, 0, 4096, 512)
                    feat_major_tiles(pc, bpc, 4, 16, conv_consumer)
                    if full or last_pre:
                        pc, bpc = piece(w_in, 0, 4608, 512)
                        feat_major_tiles(pc, bpc, 4, 20, conv_consumer)
                    ck(3)
                    pc, bpc = piece(w_in, 0, 5120, 32)
                    for tt in range(4):
                        ps, bps = fbank()
                        for k in range(8):
                            PE(lambda e, k=k, tt=tt, pc=pc, ps=ps: e.matmul(ps[:, 0:32], hT[:, k, tt * 128:(tt + 1) * 128], pc[:, k, 0:32],
                                                                            start=(k == 0), stop=(k == 7)), [bpc, b_hT], [bps])
                        DVE(lambda e, tt=tt, ps=ps: e.tensor_tensor(dtt[:, tt, :], ps[:, 0:32], dtb_row[:], ALU.add), [bps, b_dtb], [b_dtt])
                    ACT(lambda e: e.activation(dtt[:], dtt[:], AF.Exp), [b_dtt], [b_dtt])
                    ACT(lambda e: e.activation(dtt[:], dtt[:], AF.Ln, bias=1.0), [b_dtt], [b_dtt])
                    DVE(lambda e: e.tensor_tensor(aa[:], dtt[:], A_row[:].unsqueeze(1).to_broadcast([128, 4, 32]), ALU.mult), [b_dtt, b_A], [b_aa])
                    ck(4)
                    if full or last_pre:
                        pcons = make_pool_consumer(full and si == 0)
                        for pi in range(2):
                            pc, bpc = piece(w_in, 0, 5152 + pi * 512, 512)
                            feat_major_tiles(pc, bpc, 4, pi * 4, pcons)
                    if full:
                        for gi in range(2):
                            pc, bpc = piece(w_in, 0, 7200 + gi * 512, 512)
                            feat_major_tiles(pc, bpc, 4, gi * 4,
                                             lambda m, ps, bps: ACT(lambda e: e.activation(gs[:, m, :], ps[:, 0:512], AF.Sigmoid), [bps], [b_gs]))
                        pc, bpc = piece(w_pool, 0, 0, 256)
                        for g in range(4):
                            for m2 in range(2):
                                ps, bps = fbank()
                                for k2 in range(2):
                                    PE(lambda e, g=g, m2=m2, k2=k2, pc=pc, ps=ps: e.matmul(ps[:, 0:512], pc[:, g * 2 + k2, m2 * 128:(m2 + 1) * 128],
                                                                                           pooledT[:, g * 2 + k2, :], start=(k2 == 0), stop=(k2 == 1)),
                                       [bpc, b_pooledT], [bps])
                                o_ = g * 2 + m2
                                ACT(lambda e, o_=o_, ps=ps: e.activation(pT[:, o_, :], ps[:, 0:512], AF.Identity, scale=pscale[:, o_:o_ + 1]),
                                    [bps, b_pscale], [b_pT])
                        for db in range(2):
                            pc, bpc = piece(w_pool_out, 0, db * 512, 512)
                            for mt in range(4):
                                d_ = db * 4 + mt
                                ps, bps = fbank()
                                for k in range(8):
                                    PE(lambda e, k=k, mt=mt, pc=pc, ps=ps: e.matmul(ps[:, 0:512], pc[:, k, mt * 128:(mt + 1) * 128], pT[:, k, :],
                                                                                    start=(k == 0), stop=(k == 7)), [bpc, b_pT], [bps])
                                DVE(lambda e, d_=d_, ps=ps: e.tensor_tensor(mergedT[:, d_, :], ps[:, 0:512], gs[:, d_, :], ALU.mult), [bps, b_gs], [b_mergedT])
                        ck(8)
                    for cq in range(4):
                        ssd_chunk(cq, full)
                        ck(5 if not full else 9)
                    if last_pre:
                        DVE(lambda e: e.tensor_scalar(hst[:], hst[:], flag[:, 0:1], None, ALU.mult), [b_hst, b_flag], [b_hst])
                        DVE(lambda e: e.tensor_scalar(hprev[:], hprev[:], flag[:, 0:1], None, ALU.mult), [b_hprev, b_flag], [b_hprev])
                        DVE(lambda e: e.tensor_scalar(halo[:].rearrange("p m j -> p (m j)"), halo[:].rearrange("p m j -> p (m j)"), flag[:, 0:1], None, ALU.mult),
                            [b_halo, b_flag], [b_halo])
                        DVE(lambda e: e.tensor_scalar(phalo[:].rearrange("p m j -> p (m j)"), phalo[:].rearrange("p m j -> p (m j)"), flag[:, 0:1], None, ALU.mult),
                            [b_phalo, b_flag], [b_phalo])
                    if last_pre:
                        ck(7)
                    if not full:
                        ck(6)
                        continue
                    for gi in range(2):
                        pc, bpc = piece(w_in, 0, 6176 + gi * 512, 512)
                        feat_major_tiles(pc, bpc, 4, gi * 4,
                                         lambda m, ps, bps: ACT(lambda e: e.activation(gs[:, m, :], ps[:, 0:512], AF.Sigmoid), [bps], [b_gs]))
                    for db in range(2):
                        pc0, bpc0 = piece(w_ssd_out, 0, db * 512, 512)
                        pc1, bpc1 = piece(w_ssd_out, 8, db * 512, 512)
                        for mt in range(4):
                            d_ = db * 4 + mt
                            ps, bps = fbank()
                            for k in range(16):
                                pcx, bpcx = (pc0, bpc0) if k < 8 else (pc1, bpc1)
                                PE(lambda e, k=k, mt=mt, pcx=pcx, ps=ps: e.matmul(ps[:, 0:512], pcx[:, k % 8, mt * 128:(mt + 1) * 128], ynT[:, k, :],
                                                                                  start=(k == 0), stop=(k == 15)), [bpcx, b_ynT], [bps])
                            DVE(lambda e, d_=d_, ps=ps: e.tensor_tensor(acc[:], ps[:, 0:512], gs[:, d_, :], ALU.mult), [bps, b_gs], [b_acc])
                            if os.environ.get('KDBG') != 'nossd':
                                DVE(lambda e, d_=d_: e.tensor_tensor(mergedT[:, d_, :], mergedT[:, d_, :], acc[:], ALU.add), [b_mergedT, b_acc], [b_mergedT])
                    pcs = [piece(w_out, 0, cb * 512, 512) for cb in range(2)]
                    for tt in range(4):
                        xt, bxt = xt4[tt % 2]
                        r0 = si * 512 + tt * 128
                        LD(xt[:], xsrc[r0:r0 + 128, :], bxt)
                        for cb in range(2):
                            pc, bpc = pcs[cb]
                            csl = slice(cb * 512, (cb + 1) * 512)
                            ps, bps = fbank()
                            for k in range(8):
                                PE(lambda e, k=k, tt=tt, pc=pc, ps=ps: e.matmul(ps[:, 0:512], mergedT[:, k, tt * 128:(tt + 1) * 128], pc[:, k, :],
                                                                                start=(k == 0), stop=(k == 7)), [bpc, b_mergedT], [bps])
                            DVE(lambda e, ps=ps, csl=csl: e.tensor_tensor(acc[:], ps[:, 0:512], garow[:, csl], ALU.mult), [bps, b_garow], [b_acc])
                            DVE(lambda e, xt=xt, csl=csl: e.tensor_tensor(xt[:, csl], acc[:], xt[:, csl], ALU.add), [b_acc, bxt], [bxt])
                        P.dma("sp", (x1_d[r0:r0 + 128, :], xt[:]), reads=[bxt], writes=[b_x1d])
                    ck(10)

            if n_exp < 0:
                return
            ck(11)
            with QuietStack() as es:
                def esb(name, shape, dt=F32):
                    t = es.enter_context(nc.sbuf_tensor("e%d_" % state["run"] + name, list(shape), dt))
                    return t, Buf(name)

                x1, b_x1 = esb("x1", [128, 16, D])
                h2T, b_h2T = esb("h2T", [128, 8, T_CORE], BF16)
                actT32, b_actT = esb("actT32", [128, 8, T_CORE // 2])
                actT = actT32[:].bitcast(BF16)
                h32, b_h32 = actT32[:, :, 0:128], b_actT
                scr2, b_scr2 = esb("scr2", [128, D])
                st2, b_st2 = esb("st2", [128, 8])
                wr, b_wr = esb("wr", [128, 8, 32])
                brr, b_brr = esb("brr", [128, 32])
                gate, b_gate = esb("gate", [128, 16, 32])
                gateT, b_gateT = esb("gateT", [128, T_CORE])
                gpad, b_gpad = esb("gpad", [128, 128])
                lg, b_lg = esb("lg", [128, 32])
                top8, b_top8 = esb("top8", [128, 8])
                msk, b_msk = esb("msk", [128, 32])
                bgu, b_bgu = esb("bgu", [128, 32, 16])
                bdn, b_bdn = esb("bdn", [128, D])
                nfin, b_nfin = esb("nfin", [128, D])
                glu, b_glu = esb("glu", [128, 512])
                sig, b_sig = esb("sig", [128, 512])
                lin, b_lin = esb("lin", [128, 512])
                ev, b_ev = esb("ev", [128, 512])

                LD(wr[:], w_router_d.rearrange("(k p) e -> p k e", p=128), b_wr)
                LD(brr[:], brouter_row_d, b_brr)
                LD(bgu[:], bgu_col_d, b_bgu)
                LD(bdn[:], b_down_d, b_bdn)
                LD(nfin[:], nfin_row_d, b_nfin)
                DVE(lambda e: e.memset(gpad[:], 0.0), [], [b_gpad])
                for tt in range(16):
                    P.dma("sp", (x1[:, tt, :], x1_d[tt * 128:(tt + 1) * 128, :]), reads=[b_x1d], writes=[b_x1])
                for tt in range(16):
                    rms_to_T(x1[:, tt, :], b_x1, af_col, 24, h2T, b_h2T, tt * 128, scr2, b_scr2, st2, b_st2, h32=h32, bh32=b_h32)
                    ps, bps = fbank()
                    for k in range(8):
                        PE(lambda e, k=k, ps=ps: e.matmul(ps[:, 0:32], h32[:, k, :], wr[:, k, :], start=(k == 0), stop=(k == 7)), [b_h32, b_wr], [bps])
                    DVE(lambda e, ps=ps: e.tensor_tensor(lg[:], ps[:, 0:32], brr[:], ALU.add), [bps, b_brr], [b_lg])
                    DVE(lambda e: e.max(top8[:], lg[:]), [b_lg], [b_top8])
                    DVE(lambda e: e.tensor_scalar(msk[:], lg[:], top8[:, 3:4], None, ALU.is_ge), [b_lg, b_top8], [b_msk])
                    DVE(lambda e: e.tensor_scalar(lg[:], lg[:], top8[:, 0:1], None, ALU.subtract), [b_lg, b_top8], [b_lg])
                    ACT(lambda e: e.activation(lg[:], lg[:], AF.Exp), [b_lg], [b_lg])
                    DVE(lambda e: e.tensor_tensor(lg[:], lg[:], msk[:], ALU.mult), [b_lg, b_msk], [b_lg])
                    DVE(lambda e: e.reduce_sum(st2[:, 4:5], lg[:], axis=AX.X), [b_lg], [b_st2])
                    DVE(lambda e: e.reciprocal(st2[:, 5:6], st2[:, 4:5]), [b_st2], [b_st2])
                    DVE(lambda e, tt=tt: e.tensor_scalar(gate[:, tt, :], lg[:], st2[:, 5:6], None, ALU.mult), [b_lg, b_st2], [b_gate])
                    ps, bps = fbank()
                    DVE(lambda e, tt=tt: e.tensor_copy(gpad[:, 0:32], gate[:, tt, :]), [b_gate], [b_gpad])
                    PE(lambda e, ps=ps: e.transpose(ps[:, 0:128], gpad[:], ident[:]), [b_gpad, b_ident], [bps])
                    ACT(lambda e, tt=tt, ps=ps: e.activation(gateT[:, tt * 128:(tt + 1) * 128], ps[:, 0:128], AF.Identity), [bps], [b_gateT])
                ck(12)
                for tt in range(16):
                    for cb in range(2):
                        csl = slice(cb * 512, (cb + 1) * 512)
                        ps, bps = fbank()
                        PE(lambda e, tt=tt, csl=csl, ps=ps: e.matmul(ps[:, 0:512], gateT[:, tt * 128:(tt + 1) * 128], bdn[:, csl], start=True, stop=True),
                           [b_gateT, b_bdn], [bps])
                        DVE(lambda e, ps=ps, cb=cb: e.tensor_tensor(ev[:], ps[:, 0:512], garow[:, 1024 + cb * 512:1024 + (cb + 1) * 512], ALU.mult),
                            [bps, b_garow], [b_ev])
                        DVE(lambda e, tt=tt, csl=csl: e.tensor_tensor(x1[:, tt, csl], x1[:, tt, csl], ev[:], ALU.add), [b_x1, b_ev], [b_x1])
                for ex in range(n_exp):
                    for j in range(2):
                        pg_, bpg_ = piece(w_gu[ex], 0, j * 512, 512)
                        pl_, bpl_ = piece(w_gu[ex], 0, 1024 + j * 512, 512)
                        for tc in range(4):
                            tsl = slice(tc * 512, (tc + 1) * 512)
                            for mt in range(4):
                                m = j * 4 + mt
                                p1, bp1 = fbank()
                                for k in range(8):
                                    PE(lambda e, k=k, mt=mt, p1=p1, pg_=pg_, tsl=tsl: e.matmul(p1[:, 0:512], pg_[:, k, mt * 128:(mt + 1) * 128], h2T[:, k, tsl],
                                                                                               start=(k == 0), stop=(k == 7)), [bpg_, b_h2T], [bp1])
                                p2, bp2 = fbank()
                                for k in range(8):
                                    PE(lambda e, k=k, mt=mt, p2=p2, pl_=pl_, tsl=tsl: e.matmul(p2[:, 0:512], pl_[:, k, mt * 128:(mt + 1) * 128], h2T[:, k, tsl],
                                                                                               start=(k == 0), stop=(k == 7)), [bpl_, b_h2T], [bp2])
                                DVE(lambda e, p1=p1, m=m, ex=ex: e.tensor_scalar(glu[:], p1[:, 0:512], bgu[:, ex, m:m + 1], 7.0, ALU.add, ALU.min),
                                    [bp1, b_bgu], [b_glu])
                                ACT(lambda e: e.activation(sig[:], glu[:], AF.Sigmoid, scale=1.702), [b_glu], [b_sig])
                                DVE(lambda e, p2=p2, m=m, ex=ex: e.tensor_scalar(lin[:], p2[:, 0:512], bgu[:, ex, 8 + m:8 + m + 1], 7.0, ALU.add, ALU.min),
                                    [bp2, b_bgu], [b_lin])
                                DVE(lambda e: e.tensor_scalar(lin[:], lin[:], -7.0, 1.0, ALU.max, ALU.add), [b_lin], [b_lin])
                                DVE(lambda e: e.tensor_tensor(glu[:], glu[:], sig[:], ALU.mult), [b_glu, b_sig], [b_glu])
                                DVE(lambda e, m=m, tsl=tsl: e.tensor_tensor(actT[:, m, tsl], glu[:], lin[:], ALU.mult), [b_glu, b_lin], [b_actT])
                    pds = [piece(w_down[ex], 0, cb * 512, 512) for cb in range(2)]
                    for tt in range(16):
                        for cb in range(2):
                            pd_, bpd_ = pds[cb]
                            csl = slice(cb * 512, (cb + 1) * 512)
                            ps, bps = fbank()
                            for k in range(8):
                                PE(lambda e, k=k, tt=tt, ps=ps, pd_=pd_: e.matmul(ps[:, 0:512], actT[:, k, tt * 128:(tt + 1) * 128], pd_[:, k, :],
                                                                                  start=(k == 0), stop=(k == 7)), [bpd_, b_actT], [bps])
                            DVE(lambda e, ps=ps, cb=cb: e.tensor_tensor(ev[:], ps[:, 0:512], garow[:, 1024 + cb * 512:1024 + (cb + 1) * 512], ALU.mult),
                                [bps, b_garow], [b_ev])
                            DVE(lambda e, tt=tt, csl=csl, ex=ex: e.scalar_tensor_tensor(x1[:, tt, csl], ev[:], gate[:, tt, ex:ex + 1], x1[:, tt, csl],
                                                                                        ALU.mult, ALU.add), [b_ev, b_gate, b_x1], [b_x1])
                ck(13)
                for tt in range(16):
                    ACT(lambda e, tt=tt: e.activation(scr2[:], x1[:, tt, :], AF.Square), [b_x1], [b_scr2])
                    DVE(lambda e: e.reduce_sum(st2[:, 0:1], scr2[:], axis=AX.X), [b_scr2], [b_st2])
                    DVE(lambda e: e.tensor_scalar(st2[:, 1:2], st2[:, 0:1], 1.0 / D, EPS, ALU.mult, ALU.add), [b_st2], [b_st2])
                    ACT(lambda e: e.activation(st2[:, 3:4], st2[:, 1:2], AF.Sqrt), [b_st2], [b_st2])
                    DVE(lambda e: e.reciprocal(st2[:, 2:3], st2[:, 3:4]), [b_st2], [b_st2])
                    DVE(lambda e, tt=tt: e.scalar_tensor_tensor(scr2[:], x1[:, tt, :], st2[:, 2:3], nfin[:], ALU.mult, ALU.mult),
                        [b_x1, b_st2, b_nfin], [b_scr2])
                    P.dma("sp", (out_d[tt * 128:(tt + 1) * 128, :], scr2[:]), reads=[b_scr2], is_output=True)

        b_x1d = Buf("x1d")
        Pd = Prog(nc, st, dry=True)
        try:
            program(Pd)
        except _Stop:
            pass
        P = Prog(nc, st)
        try:
            program(P)
        except _Stop:
            pass
        with nc.Block() as block:
            P.emit(block)
        _LAST['seq'] = dict(P.seq)
    return nc


def _col(v, n):
    return np.ascontiguousarray(np.asarray(v, np.float32).reshape(n, 128).T)


def _row(v):
    v = np.asarray(v, np.float32).reshape(1, -1)
    return np.ascontiguousarray(np.broadcast_to(v, (128, v.shape[1])))


def make_in_maps(x, c, w_ada, b_ada, norm_mix_w, w_in, conv_w, conv_b, dt_bias, a_log, d_skip,
                 ssd_norm_w, w_ssd_out, w_pool, pool_scale, w_pool_out, w_out, norm_ffn_w,
                 w_router, b_router, w_gu, b_gu, w_down, b_down, norm_final_w):
    f = lambda a: np.ascontiguousarray(np.asarray(a, np.float32))
    x = f(x)
    ident = np.eye(128, dtype=np.float32)
    utri = np.triu(np.ones((128, 128), np.float32))
    negm = np.where(np.arange(128)[None, :] < np.arange(128)[:, None], NEG, 0.0).astype(np.float32)
    negm4 = np.ascontiguousarray(np.tile(negm, (1, 4)))
    shared = dict(
        w_ada=f(w_ada[0]), bada_col=_col(b_ada[0], 48),
        bada_row=np.ascontiguousarray(np.concatenate([_row(b_ada[0][2048:3072]), _row(b_ada[0][5120:6144])], axis=1)),
        nmw_col=_col(norm_mix_w[0], 8), nfw_col=_col(norm_ffn_w[0], 8), nfin_row=_row(norm_final_w),
        w_in=f(w_in[0]),
        convw_col=np.ascontiguousarray(np.asarray(conv_w[0], np.float32).reshape(4, 24, 128).transpose(2, 1, 0)),
        convb_col=_col(conv_b[0], 24),
        dtb_row=_row(dt_bias[0]), alog_row=_row(a_log[0]), dskip_row=_row(d_skip[0]),
        ssdnw_col=_col(ssd_norm_w[0], 16), w_ssd_out=f(w_ssd_out[0]),
        w_pool=f(np.asarray(w_pool[0]).reshape(1024, 256)), pscale_col=_col(pool_scale[0], 8),
        w_pool_out=f(w_pool_out[0]), w_out=f(w_out[0]),
        w_router=f(w_router[0]), brouter_row=_row(b_router[0]),
        w_gu=f(w_gu[0]), bgu_col=np.ascontiguousarray(np.asarray(b_gu[0], np.float32).reshape(32, 16, 128).transpose(2, 0, 1)),
        w_down=f(w_down[0]), b_down=np.ascontiguousarray(np.concatenate([f(b_down[0]), np.zeros((96, 1024), np.float32)], 0)),
        ident=ident, utri=utri, negm4=negm4,
    )
    maps = []
    for core in range(8):
        b, hf = core // 2, core % 2
        inv = np.empty((4, 512), np.float32)
        t = np.arange(512)
        for wi, w in enumerate((2, 4, 8, 16)):
            inv[wi] = 1.0 / (np.minimum(t + 1, w) if hf == 0 else w)
        m = dict(shared)
        m["x_own"] = np.ascontiguousarray(x[b, hf * 2048:(hf + 1) * 2048])
        m["x_pre"] = np.ascontiguousarray(x[b, 0:2048])
        m["flag"] = np.full((128, 1), float(hf), np.float32)
        m["invcnt"] = np.ascontiguousarray(np.broadcast_to(inv[None], (128, 4, 512)))
        m["c_col"] = _col(np.asarray(c, np.float32)[b], 8)
        maps.append(m)
    return maps


_NC_CACHE = {}


def kernel(**inputs):
    if "nc" not in _NC_CACHE:
        _NC_CACHE["nc"] = build_nc()
    nc = _NC_CACHE["nc"]
    maps = make_in_maps(**inputs)
    res = run_bass_kernel_spmd(nc, maps, core_ids=list(range(8)))
    out = np.empty((4, 4096, 1024), np.float32)
    for core in range(8):
        b, hf = core // 2, core % 2
        out[b, hf * 2048:(hf + 1) * 2048] = res.results[core]["out"]
    return out
```

```python
import os
import numpy as np
from contextlib import ExitStack
import concourse.bass as bass
import concourse.mybir as mybir
from concourse.bass_utils import run_bass_kernel_spmd

F32 = mybir.dt.float32
BF16 = mybir.dt.bfloat16
AF = mybir.ActivationFunctionType
ALU = mybir.AluOpType
AX = mybir.AxisListType

ENGS = ("pe", "act", "dve", "pool", "sp")
EPS = 1e-5
NEG = -30000.0


SEM_EPOCH = 2000


class SemBox:
    __slots__ = ("sem",)

    def __init__(self, sem):
        self.sem = sem


class Buf:
    __slots__ = ("name", "w", "r", "box", "cnt")

    def __init__(self, name):
        self.name = name
        self.w = None
        self.r = []
        self.box = None
        self.cnt = 0


class Prog:
    def __init__(self, nc, stack, dry=False):
        self.nc = nc
        self.stack = stack
        self.dry = dry
        self.ops = {e: [] for e in ENGS}
        self.seq = {e: 0 for e in ENGS}
        self.waited = {e: {} for e in ENGS}
        self.esem = {e: [] for e in ENGS}
        self.nsem = 0
        self.out_tokens = []

    def _esem(self, eng, n):
        e = (n - 1) // SEM_EPOCH
        while len(self.esem[eng]) <= e:
            self.esem[eng].append(self.stack.enter_context(self.nc.semaphore("es_%s_%d" % (eng, len(self.esem[eng])))))
        return self.esem[eng][e], (n - 1) % SEM_EPOCH + 1

    def _sem_of(self, key, v):
        if isinstance(key, str):
            return self._esem(key, v)
        return key.sem, v

    def _waits(self, eng, reads, writes):
        need = {}
        for b in reads:
            if b.w is not None:
                k, v = b.w
                need[k] = max(need.get(k, 0), v)
        for b in writes:
            if b.w is not None:
                k, v = b.w
                need[k] = max(need.get(k, 0), v)
            for (k, v) in b.r:
                need[k] = max(need.get(k, 0), v)
        out = []
        wd = self.waited[eng]
        for k, v in need.items():
            if k == "pe" and eng == "pe":
                continue
            if wd.get(k, 0) >= v:
                continue
            wd[k] = v
            out.append((k, v))
        return out

    def op(self, eng, fn, reads=(), writes=()):
        if self.dry:
            return
        waits = self._waits(eng, reads, writes)
        self.seq[eng] += 1
        tok = (eng, self.seq[eng])
        for b in reads:
            b.r.append(tok)
            if len(b.r) > 64:
                best = {}
                for (k, v) in b.r:
                    best[k] = max(best.get(k, 0), v)
                b.r = list(best.items())
        for b in writes:
            b.w = tok
            b.r = []
        self.ops[eng].append((waits, fn, None))

    def dma(self, eng, pairs, reads=(), writes=(), is_output=False):
        if self.dry:
            return
        if not isinstance(pairs, list):
            pairs = [pairs]
        owner = writes[0] if writes else reads[0]
        if owner.box is None or owner.cnt + 16 * len(pairs) > SEM_EPOCH:
            owner.box = SemBox(self.stack.enter_context(self.nc.semaphore("ds_%d" % self.nsem)))
            owner.cnt = 0
            self.nsem += 1
        waits = self._waits(eng, reads, writes)
        owner.cnt += 16 * len(pairs)
        box = owner.box
        tok = (box, owner.cnt)
        for b in reads:
            b.r.append(tok)
        for b in writes:
            b.w = tok
            b.r = []
        for i, (o, a) in enumerate(pairs):
            self.ops[eng].append((waits if i == 0 else [], None, (o, a, box)))
        if is_output:
            self.out_tokens.append(tok)

    def emit(self, block):
        def run(eng, h):
            n = 0
            for waits, fn, d in self.ops[eng]:
                for k, v in waits:
                    sem, val = self._sem_of(k, v)
                    h.wait_ge(sem, val)
                if fn is not None:
                    n += 1
                    sem, _ = self._esem(eng, n)
                    fn(h).then_inc(sem, 1)
                else:
                    o, a, box = d
                    h.dma_start(out=o, in_=a).then_inc(box.sem, 16)
            if eng == "sp":
                for k, v in self.out_tokens:
                    sem, val = self._sem_of(k, v)
                    h.wait_ge(sem, val)

        @block.tensor
        def _(h):
            run("pe", h)

        @block.scalar
        def _(h):
            run("act", h)

        @block.vector
        def _(h):
            run("dve", h)

        @block.gpsimd
        def _(h):
            run("pool", h)

        @block.sync
        def _(h):
            run("sp", h)


T_CORE = 2048
D = 1024
NSC = 4
RING = 4
N_EXP = 32
DEBUG_X1 = False


_LAST = {}


class _Stop(Exception):
    pass


class QuietStack(ExitStack):
    def __exit__(self, et, ev, tb):
        super().__exit__(None, None, None)
        return False


def build_nc(n_exp=N_EXP, debug=False, stop=0, n_w=32):
    nc = bass.Bass("TRN2", target_bir_lowering=False)

    def din(name, shape, dt=F32):
        return nc.dram_tensor(name, list(shape), dt, kind="ExternalInput").ap()

    x_own = din("x_own", [T_CORE, D])
    x_pre = din("x_pre", [T_CORE, D])
    flag_d = din("flag", [128, 1])
    invcnt_d = din("invcnt", [128, 4, 512])
    c_col_d = din("c_col", [128, 8])
    w_ada = din("w_ada", [D, 6 * D])
    bada_col_d = din("bada_col", [128, 48])
    bada_row_d = din("bada_row", [128, 2048])
    nmw_col_d = din("nmw_col", [128, 8])
    nfw_col_d = din("nfw_col", [128, 8])
    nfin_row_d = din("nfin_row", [128, D])
    w_in = din("w_in", [D, 8224])
    convw_col_d = din("convw_col", [128, 24, 4])
    convb_col_d = din("convb_col", [128, 24])
    dtb_row_d = din("dtb_row", [128, 32])
    alog_row_d = din("alog_row", [128, 32])
    dskip_row_d = din("dskip_row", [128, 32])
    ssdnw_col_d = din("ssdnw_col", [128, 16])
    w_ssd_out = din("w_ssd_out", [2048, D])
    w_pool = din("w_pool", [1024, 256])
    pscale_col_d = din("pscale_col", [128, 8])
    w_pool_out = din("w_pool_out", [D, D])
    w_out = din("w_out", [D, D])
    w_router_d = din("w_router", [D, 32])
    brouter_row_d = din("brouter_row", [128, 32])
    w_gu = din("w_gu", [n_w, D, 2048])
    bgu_col_d = din("bgu_col", [128, 32, 16])
    w_down = din("w_down", [n_w, D, D])
    b_down_d = din("b_down", [128, D])
    ident_d = din("ident", [128, 128])
    utri_d = din("utri", [128, 128])
    negm4_d = din("negm4", [128, 512])
    out_d = nc.dram_tensor("out", [T_CORE, D], F32, kind="ExternalOutput").ap()
    x1_d = nc.dram_tensor("x1_scratch", [T_CORE, D], F32, kind=("ExternalOutput" if debug else "Internal")).ap()

    with ExitStack() as st:
        def sb(name, shape, dt=F32):
            t = st.enter_context(nc.sbuf_tensor("s_" + name, list(shape), dt))
            return t, Buf(name)

        def psum(name, shape, dt=F32):
            t = st.enter_context(nc.psum_tensor(name, list(shape), dt))
            return t, Buf(name)

        ident, b_ident = sb("ident", [128, 128])
        identb, b_identb = sb("identb", [128, 128], BF16)
        utri, b_utri = sb("utri", [128, 128])
        ones32, b_ones32 = sb("ones32", [128, 128])
        negm4, b_negm4 = sb("negm4", [128, 512])
        flag, b_flag = sb("flag", [128, 1])
        c_col, b_ccol = sb("c_col", [128, 8])
        c_act, b_cact = sb("c_act", [128, 8], BF16)
        cbc, b_cbc = sb("cbc", [128, 8, 128], BF16)
        bada_col, b_badac = sb("bada_col", [128, 48])
        modcol, b_modcol = sb("modcol", [128, 48])
        garow, b_garow = sb("garow", [128, 2048])
        nmw_col, b_nmw = sb("nmw_col", [128, 8])
        nfw_col, b_nfw = sb("nfw_col", [128, 8])
        am_col, b_am = sb("am_col", [128, 8])
        af_col, b_af = sb("af_col", [128, 8])
        convw, b_convw = sb("convw", [128, 24, 4])
        convb, b_convb = sb("convb", [128, 24])
        dtb_row, b_dtb = sb("dtb_row", [128, 32])
        A_row, b_A = sb("A_row", [128, 32])
        dskip_row, b_dskip = sb("dskip_row", [128, 32])
        pscale, b_pscale = sb("pscale", [128, 8])
        PARAMS = [b_ident, b_identb, b_utri, b_ones32, b_negm4, b_flag, b_cact, b_cbc, b_modcol, b_garow,
                  b_am, b_af, b_convw, b_convb, b_dtb, b_A, b_dskip, b_pscale]

        fbanks = [psum("pf%d" % i, [128, 512]) for i in range(6)]
        tbanks = [psum("pt%d" % i, [128, 1024], BF16) for i in range(2)]
        rr = {"f": 0, "t": 0}

        def fbank():
            r = fbanks[rr["f"] % 6]
            rr["f"] += 1
            return r

        def tbank():
            r = tbanks[rr["t"] % 2]
            rr["t"] += 1
            return r

        ring = [sb("ring%d" % i, [128, 8, 512], BF16) for i in range(RING)]

        plan = []
        state = {"P": None, "issued": 0, "taken": 0, "run": 0}

        def piece(W2d, k0, c0, w):
            P = state["P"]
            if P.dry:
                plan.append((W2d, k0, c0, w))
                return ring[0][0], ring[0][1]
            i = state["taken"]
            assert plan[i][1:] == (k0, c0, w), (i, plan[i][1:], (k0, c0, w))
            while state["issued"] < min(len(plan), i + RING - 1):
                j = state["issued"]
                Wj, kj, cj, wj = plan[j]
                t, b = ring[j % RING]
                src = Wj[kj * 128:(kj + 8) * 128, cj:cj + wj].rearrange("(k p) c -> p k c", p=128)
                P.dma("pool", (t[:, :, 0:wj], src), writes=[b])
                state["issued"] += 1
            state["taken"] += 1
            return ring[i % RING]

        def program(P):
            state["P"] = P
            state["run"] += 1
            state["issued"] = 0
            state["taken"] = 0
            rr["f"] = 0
            rr["t"] = 0

            def ck(n):
                if stop == n:
                    raise _Stop()

            def PE(fn, r, w): P.op("pe", fn, r, w)
            def ACT(fn, r, w): P.op("act", fn, r, w)
            def DVE(fn, r, w): P.op("dve", fn, r, w)
            def LD(dst, src, b): P.dma("sp", (dst, src), writes=[b])

            LD(ident[:], ident_d, b_ident)
            LD(utri[:], utri_d, b_utri)
            LD(negm4[:], negm4_d, b_negm4)
            LD(flag[:], flag_d, b_flag)
            LD(c_col[:], c_col_d, b_ccol)
            LD(bada_col[:], bada_col_d, b_badac)
            LD(garow[:], bada_row_d, b_garow)
            LD(nmw_col[:], nmw_col_d, b_nmw)
            LD(nfw_col[:], nfw_col_d, b_nfw)
            LD(convw[:], convw_col_d, b_convw)
            LD(convb[:], convb_col_d, b_convb)
            LD(dtb_row[:], dtb_row_d, b_dtb)
            LD(A_row[:], alog_row_d, b_A)
            LD(dskip_row[:], dskip_row_d, b_dskip)
            LD(pscale[:], pscale_col_d, b_pscale)
            DVE(lambda e: e.tensor_copy(identb[:], ident[:]), [b_ident], [b_identb])
            DVE(lambda e: e.memset(ones32[:], 1.0), [], [b_ones32])
            ACT(lambda e: e.activation(A_row[:], A_row[:], AF.Exp), [b_A], [b_A])
            DVE(lambda e: e.tensor_scalar(A_row[:], A_row[:], -1.0, None, ALU.mult), [b_A], [b_A])
            ACT(lambda e: e.activation(c_act[:], c_col[:], AF.Silu), [b_ccol], [b_cact])
            DVE(lambda e: e.tensor_copy(cbc[:], c_act[:].unsqueeze(2).to_broadcast([128, 8, 128])), [b_cact], [b_cbc])

            for j in range(12):
                pc, bpc = piece(w_ada, 0, j * 512, 512)
                ps, bps = fbank()
                if j in (4, 5, 10, 11):
                    half = {4: 0, 5: 1, 10: 2, 11: 3}[j]
                    for k in range(8):
                        PE(lambda e, k=k, pc=pc, ps=ps: e.matmul(ps[:, 0:512], cbc[:, k, :], pc[:, k, :], start=(k == 0), stop=(k == 7)),
                           [b_cbc, bpc], [bps])
                    DVE(lambda e, ps=ps, half=half: e.tensor_tensor(garow[:, half * 512:(half + 1) * 512], ps[:, 0:512],
                                                                    garow[:, half * 512:(half + 1) * 512], ALU.add), [bps, b_garow], [b_garow])
                else:
                    for mt in range(4):
                        for k in range(8):
                            PE(lambda e, k=k, mt=mt, pc=pc, ps=ps: e.matmul(ps[:, mt:mt + 1], pc[:, k, mt * 128:(mt + 1) * 128], c_act[:, k:k + 1],
                                                                            start=(k == 0), stop=(k == 7)), [b_cact, bpc], [bps])
                    DVE(lambda e, ps=ps, j=j: e.tensor_tensor(modcol[:, j * 4:(j + 1) * 4], ps[:, 0:4], bada_col[:, j * 4:(j + 1) * 4], ALU.add),
                        [bps, b_badac], [b_modcol])
            DVE(lambda e: e.scalar_tensor_tensor(am_col[:], modcol[:, 8:16], 1.0, nmw_col[:], ALU.add, ALU.mult), [b_modcol, b_nmw], [b_am])
            DVE(lambda e: e.scalar_tensor_tensor(af_col[:], modcol[:, 32:40], 1.0, nfw_col[:], ALU.add, ALU.mult), [b_modcol, b_nfw], [b_af])
            ck(1)

            def rms_to_T(xt, bxt, a_col, s_col0, dstT, bdst, col0, scr, bscr, st_, bst, h32=None, bh32=None):
                ACT(lambda e: e.activation(scr[:, 0:D], xt, AF.Square), [bxt], [bscr])
                DVE(lambda e: e.reduce_sum(st_[:, 0:1], scr[:, 0:D], axis=AX.X), [bscr], [bst])
                DVE(lambda e: e.tensor_scalar(st_[:, 1:2], st_[:, 0:1], 1.0 / D, EPS, ALU.mult, ALU.add), [bst], [bst])
                ACT(lambda e: e.activation(st_[:, 3:4], st_[:, 1:2], AF.Sqrt), [bst], [bst])
                DVE(lambda e: e.reciprocal(st_[:, 2:3], st_[:, 3:4]), [bst], [bst])
                ACT(lambda e: e.activation(scr[:, 0:D], xt, AF.Identity, scale=st_[:, 2:3]), [bxt, bst], [bscr])
                for hh in range(2):
                    ps, bps = fbank()
                    for q in range(4):
                        k = hh * 4 + q
                        PE(lambda e, k=k, q=q, ps=ps: e.transpose(ps[:, q * 128:(q + 1) * 128], scr[:, k * 128:(k + 1) * 128], ident[:]),
                           [bscr, b_ident], [bps])
                    for q in range(4):
                        k = hh * 4 + q
                        ACT(lambda e, k=k, q=q, ps=ps: e.activation(dstT[:, k, col0:col0 + 128], ps[:, q * 128:(q + 1) * 128], AF.Identity,
                                                                    bias=modcol[:, s_col0 + k:s_col0 + k + 1], scale=a_col[:, k:k + 1]),
                            [bps, b_modcol, b_am, b_af], [bdst])
                        if h32 is not None:
                            ACT(lambda e, k=k, q=q, ps=ps: e.activation(h32[:, k, :], ps[:, q * 128:(q + 1) * 128], AF.Identity,
                                                                        bias=modcol[:, s_col0 + k:s_col0 + k + 1], scale=a_col[:, k:k + 1]),
                                [bps, b_modcol, b_am, b_af], [bh32])

            with QuietStack() as ms:
                def msb(name, shape, dt=F32):
                    t = ms.enter_context(nc.sbuf_tensor("m%d_" % state["run"] + name, list(shape), dt))
                    return t, Buf(name)

                xt4 = [msb("xt%d" % i, [128, D]) for i in range(2)]
                scr, b_scr = msb("scr", [128, D])
                stt, b_stt = msb("stt", [128, 8])
                hT, b_hT = msb("hT", [128, 8, 512], BF16)
                zs, b_zs = msb("zs", [128, 4, 2048], BF16)
                xc, b_xc = msb("xc", [128, 24, 512], BF16)
                ub = [msb("ub%d" % i, [128, 515]) for i in range(2)]
                acc, b_acc = msb("acc", [128, 512])
                halo, b_halo = msb("halo", [128, 24, 3])
                pb, b_pb = msb("pb", [128, 527])
                ps1, b_ps1 = msb("ps1", [128, 527])
                ps2, b_ps2 = msb("ps2", [128, 527])
                phalo, b_phalo = msb("phalo", [128, 8, 15])
                ivt, b_ivt = msb("ivt", [128, 512])
                big16, b_big = msb("big16", [128, 16, 512], BF16)
                pooledT, b_pooledT = big16[:, 0:8, :], b_big
                pT, b_pT = big16[:, 8:16, :], b_big
                ynT, b_ynT = big16, b_big
                dtt, b_dtt = msb("dtt", [128, 4, 32])
                aa, b_aa = msb("aa", [128, 4, 32])
                gs, b_gs = msb("gs", [128, 8, 512], BF16)
                mergedT, b_mergedT = msb("mergedT", [128, 8, 512], BF16)
                hst, b_hst = msb("hst", [128, 2048])
                hprev, b_hprev = msb("hprev", [128, 2048], BF16)
                ssdnw, b_ssdnw = msb("ssdnw", [128, 16])
                sm, b_sm = msb("sm", [128, 8, 4, 32])
                b_sm1, b_sm2, b_sm3, b_sm4, b_sm5, b_sm6 = [Buf("sm%d" % i) for i in range(1, 7)]
                Xb, b_Xb = msb("Xb", [128, 2048], BF16)
                Xw, b_Xw = msb("Xw", [128, 2048], BF16)
                ynb, b_ynb = Xw, b_Xw
                Bt, b_Bt = msb("Bt", [128, 512], BF16)
                yk, b_yk = msb("yk", [128, 2048])
                cbT2 = [msb("cbT%d" % i, [128, 128]) for i in range(2)]
                aU2 = [msb("aU%d" % i, [128, 4, 128]) for i in range(2)]
                dec2 = [msb("dec%d" % i, [128, 4, 128]) for i in range(2)]
                MT2 = [msb("MT%d" % i, [128, 4, 128], BF16) for i in range(2)]
                tmpg2 = [msb("tmpg%d" % i, [128, 512]) for i in range(2)]
                acc2 = [msb("acc%d" % i, [128, 512]) for i in range(2)]
                rot = {"g": 0, "q": 0, "c": 0}
                ssg, b_ssg = msb("ssg", [128, 8])

                LD(ssdnw[:], ssdnw_col_d, b_ssdnw)
                DVE(lambda e: e.memset(halo[:], 0.0), [], [b_halo])
                DVE(lambda e: e.memset(ps1[:], 0.0), [], [b_ps1])
                DVE(lambda e: e.memset(ps2[:], 0.0), [], [b_ps2])
                DVE(lambda e: e.memset(phalo[:], 0.0), [], [b_phalo])
                DVE(lambda e: e.memset(hst[:], 0.0), [], [b_hst])
                DVE(lambda e: e.memset(hprev[:], 0.0), [], [b_hprev])

                def feat_major_tiles(pc, bpc, n_mt, m0, consumer):
                    for mt in range(n_mt):
                        ps, bps = fbank()
                        for k in range(8):
                            PE(lambda e, k=k, mt=mt, pc=pc, ps=ps: e.matmul(ps[:, 0:512], pc[:, k, mt * 128:(mt + 1) * 128], hT[:, k, :],
                                                                            start=(k == 0), stop=(k == 7)), [bpc, b_hT], [bps])
                        consumer(m0 + mt, ps, bps)

                ubi = {"i": 0}

                def conv_consumer(m, ps, bps):
                    u, bu = ub[ubi["i"] % 2]
                    acc, b_acc = acc2[ubi["i"] % 2]
                    ubi["i"] += 1
                    ACT(lambda e: e.activation(u[:, 3:515], ps[:, 0:512], AF.Identity), [bps], [bu])
                    DVE(lambda e: e.tensor_copy(u[:, 0:3], halo[:, m, :]), [b_halo], [bu])
                    DVE(lambda e: e.tensor_copy(halo[:, m, :], u[:, 512:515]), [bu], [b_halo])
                    DVE(lambda e: e.tensor_scalar(acc[:], u[:, 0:512], convw[:, m, 0:1], None, ALU.mult), [bu, b_convw], [b_acc])
                    for j in range(1, 4):
                        DVE(lambda e, j=j: e.scalar_tensor_tensor(acc[:], u[:, j:j + 512], convw[:, m, j:j + 1], acc[:], ALU.mult, ALU.add),
                            [bu, b_convw, b_acc], [b_acc])
                    ACT(lambda e: e.activation(xc[:, m, :], acc[:], AF.Silu, bias=convb[:, m:m + 1]), [b_acc, b_convb], [b_xc])

                def make_pool_consumer(first_own):
                    def pool_consumer(m, ps, bps):
                        wi = m // 2
                        w = (2, 4, 8, 16)[wi]
                        ACT(lambda e: e.activation(pb[:, 15:527], ps[:, 0:512], AF.Identity), [bps], [b_pb])
                        DVE(lambda e: e.tensor_copy(pb[:, 0:15], phalo[:, m, :]), [b_phalo], [b_pb])
                        DVE(lambda e: e.tensor_copy(phalo[:, m, :], pb[:, 512:527]), [b_pb], [b_phalo])
                        cur, bcur = pb, b_pb
                        nxt = [(ps1, b_ps1), (ps2, b_ps2)]
                        ni = 0
                        sh = 1
                        while sh < w:
                            nt, bnt = nxt[ni % 2]
                            ni += 1
                            DVE(lambda e, nt=nt, cur=cur, sh=sh: e.tensor_tensor(nt[:, sh:527], cur[:, sh:527], cur[:, 0:527 - sh], ALU.add),
                                [bcur], [bnt])
                            cur, bcur = nt, bnt
                            sh *= 2
                        if first_own:
                            LD(ivt[:], invcnt_d[:, wi, :], b_ivt)
                            DVE(lambda e, cur=cur: e.tensor_tensor(acc[:], cur[:, 15:527], ivt[:], ALU.mult), [bcur, b_ivt], [b_acc])
                            DVE(lambda e: e.tensor_tensor(pooledT[:, m, :], acc[:], pb[:, 15:527], ALU.subtract), [b_acc, b_pb], [b_pooledT])
                        else:
                            DVE(lambda e, cur=cur: e.scalar_tensor_tensor(pooledT[:, m, :], cur[:, 15:527], 1.0 / w, pb[:, 15:527], ALU.mult, ALU.subtract),
                                [bcur, b_pb], [b_pooledT])
                    return pool_consumer

                def ssd_prep():
                    ps, bps = fbank()
                    for cq in range(4):
                        PE(lambda e, cq=cq, ps=ps: e.matmul(ps[:, cq * 32:(cq + 1) * 32], utri[:], aa[:, cq, :], start=True, stop=True), [b_utri, b_aa], [bps])
                        PE(lambda e, cq=cq, ps=ps: e.matmul(ps[:, 128 + cq * 32:128 + (cq + 1) * 32], ones32[:], aa[:, cq, :], start=True, stop=True),
                           [b_ones32, b_aa], [bps])
                    f = lambda r: sm[:, r, :, :].rearrange("p c h -> p (c h)")
                    DVE(lambda e: e.tensor_copy(f(0), ps[:, 0:128]), [bps], [b_sm])
                    DVE(lambda e: e.tensor_scalar(f(1), ps[:, 0:128], -1.0, None, ALU.mult), [bps], [b_sm1])
                    DVE(lambda e: e.tensor_copy(f(2), ps[:, 128:256]), [bps], [b_sm2])
                    DVE(lambda e: e.tensor_tensor(f(6), f(2), f(0), ALU.subtract), [b_sm, b_sm2], [b_sm6])
                    ACT(lambda e: e.activation(f(3), f(0), AF.Exp), [b_sm], [b_sm3])
                    ACT(lambda e: e.activation(f(4), f(6), AF.Exp), [b_sm6], [b_sm4])
                    ACT(lambda e: e.activation(f(5), f(2), AF.Exp), [b_sm2], [b_sm5])

                def ssd_chunk(cq, full):
                    cs = cq * 128
                    for hh in range(2):
                        pt, bpt = tbank()
                        for q in range(8):
                            m = hh * 8 + q
                            PE(lambda e, m=m, q=q, pt=pt: e.transpose(pt[:, q * 128:(q + 1) * 128], xc[:, m, cs:cs + 128], identb[:]),
                               [b_xc, b_identb], [bpt])
                        sl = slice(hh * 1024, (hh + 1) * 1024)
                        hsl = slice(hh * 16, (hh + 1) * 16)
                        DVE(lambda e, pt=pt, sl=sl, hsl=hsl: e.tensor_tensor(
                            Xb[:, sl].rearrange("p (h d) -> p h d", h=16), pt[:].rearrange("p (h d) -> p h d", h=16),
                            dtt[:, cq, hsl].unsqueeze(2).to_broadcast([128, 16, 64]), ALU.mult), [bpt, b_dtt], [b_Xb])
                        if full:
                            DVE(lambda e, pt=pt, sl=sl, hsl=hsl: e.tensor_tensor(
                                yk[:, sl].rearrange("p (h d) -> p h d", h=16), pt[:].rearrange("p (h d) -> p h d", h=16),
                                dskip_row[:, hsl].unsqueeze(2).to_broadcast([128, 16, 64]), ALU.mult), [bpt, b_dskip], [b_yk])
                    pt, bpt = tbank()
                    for g in range(4):
                        PE(lambda e, g=g, pt=pt: e.transpose(pt[:, g * 128:(g + 1) * 128], xc[:, 16 + g, cs:cs + 128], identb[:]),
                           [b_xc, b_identb], [bpt])
                    ACT(lambda e, pt=pt: e.activation(Bt[:], pt[:, 0:512], AF.Identity), [bpt], [b_Bt])
                    DVE(lambda e: e.tensor_tensor(Xw[:].rearrange("p (h d) -> p h d", h=32), Xb[:].rearrange("p (h d) -> p h d", h=32),
                                                  sm[:, 4, cq, :].unsqueeze(2).to_broadcast([128, 32, 64]), ALU.mult), [b_Xb, b_sm4], [b_Xw])
                    for g in range(4):
                        gsl = slice(g * 512, (g + 1) * 512)
                        if full:
                            cbT, b_cbT = cbT2[rot["g"] % 2]
                            tmpg, b_tmpg = tmpg2[rot["g"] % 2]
                            rot["g"] += 1
                            pcb, bpcb = fbank()
                            PE(lambda e, pcb=pcb, g=g: e.matmul(pcb[:, 0:128], xc[:, 16 + g, cs:cs + 128], xc[:, 20 + g, cs:cs + 128], start=True, stop=True),
                               [b_xc], [bpcb])
                            ACT(lambda e, pcb=pcb, cbT=cbT: e.activation(cbT[:], pcb[:, 0:128], AF.Identity), [bpcb], [b_cbT])
                            py, bpy = fbank()
                            for hq in range(2):
                                h0 = g * 8 + hq * 4
                                aU, b_aU = aU2[rot["q"] % 2]
                                dec, b_dec = dec2[rot["q"] % 2]
                                MT, b_MT = MT2[rot["q"] % 2]
                                rot["q"] += 1
                                DVE(lambda e, h0=h0, aU=aU: e.tensor_tensor(aU[:], utri[:].unsqueeze(1).to_broadcast([128, 4, 128]),
                                                                     aa[:, cq, h0:h0 + 4].unsqueeze(2).to_broadcast([128, 4, 128]), ALU.mult),
                                    [b_utri, b_aa], [b_aU])
                                pg, bpg = fbank()
                                PE(lambda e, pg=pg, aU=aU: e.matmul(pg[:, 0:512], ones32[:], aU[:].rearrange("p h l -> p (h l)"), start=True, stop=False),
                                   [b_ones32, b_aU], [bpg])
                                PE(lambda e, pg=pg: e.matmul(pg[:, 0:512], ident[:], negm4[:], start=False, stop=True), [b_ident, b_negm4], [bpg])
                                for j in range(4):
                                    ACT(lambda e, j=j, h0=h0, pg=pg, dec=dec: e.activation(dec[:, j, :], pg[:, j * 128:(j + 1) * 128], AF.Exp,
                                                                                           bias=sm[:, 1, cq, h0 + j:h0 + j + 1]), [bpg, b_sm1], [b_dec])
                                DVE(lambda e, MT=MT, dec=dec, cbT=cbT: e.tensor_tensor(MT[:], dec[:], cbT[:].unsqueeze(1).to_broadcast([128, 4, 128]), ALU.mult),
                                    [b_dec, b_cbT], [b_MT])
                                for j in range(4):
                                    h = h0 + j
                                    c0 = (hq * 4 + j) * 64
                                    PE(lambda e, j=j, h=h, c0=c0, py=py, MT=MT: e.matmul(py[:, c0:c0 + 64], MT[:, j, :], Xb[:, h * 64:(h + 1) * 64],
                                                                                  start=True, stop=True), [b_MT, b_Xb], [bpy])
                            po, bpo = fbank()
                            PE(lambda e, po=po, gsl=gsl, g=g: e.matmul(po[:, 0:512], xc[:, 20 + g, cs:cs + 128], hprev[:, gsl], start=True, stop=True),
                               [b_xc, b_hprev], [bpo])
                            DVE(lambda e, po=po, g=g, tmpg=tmpg: e.tensor_tensor(tmpg[:].rearrange("p (h d) -> p h d", h=8), po[:].rearrange("p (h d) -> p h d", h=8),
                                                                 sm[:, 3, cq, g * 8:(g + 1) * 8].unsqueeze(2).to_broadcast([128, 8, 64]), ALU.mult),
                                [bpo, b_sm3], [b_tmpg])
                            DVE(lambda e, py=py, tmpg=tmpg: e.tensor_tensor(tmpg[:], tmpg[:], py[:, 0:512], ALU.add), [b_tmpg, bpy], [b_tmpg])
                            DVE(lambda e, gsl=gsl, tmpg=tmpg: e.tensor_tensor(yk[:, gsl], yk[:, gsl], tmpg[:], ALU.add), [b_yk, b_tmpg], [b_yk])
                        pst, bpst = fbank()
                        PE(lambda e, pst=pst, gsl=gsl, g=g: e.matmul(pst[:, 0:512], Bt[:, g * 128:(g + 1) * 128], Xw[:, gsl], start=True, stop=True),
                           [b_Bt, b_Xw], [bpst])
                        DVE(lambda e, gsl=gsl, g=g: e.tensor_tensor(hst[:, gsl].rearrange("p (h d) -> p h d", h=8), hst[:, gsl].rearrange("p (h d) -> p h d", h=8),
                                                               sm[:, 5, cq, g * 8:(g + 1) * 8].unsqueeze(2).to_broadcast([128, 8, 64]), ALU.mult),
                            [b_hst, b_sm5], [b_hst])
                        DVE(lambda e, pst=pst, gsl=gsl: e.tensor_tensor(hst[:, gsl], hst[:, gsl], pst[:, 0:512], ALU.add), [b_hst, bpst], [b_hst])
                        ACT(lambda e, gsl=gsl: e.activation(hprev[:, gsl], hst[:, gsl], AF.Identity), [b_hst], [b_hprev])
                    if not full or os.environ.get('KDBG') == 'nossd':
                        return
                    DVE(lambda e: e.tensor_tensor(yk[:], yk[:], zs[:, cq, :], ALU.mult), [b_yk, b_zs], [b_yk])
                    for hh in range(2):
                        ACT(lambda e, hh=hh: e.activation(scr[:, 0:D], yk[:, hh * 1024:(hh + 1) * 1024], AF.Square), [b_yk], [b_scr])
                        DVE(lambda e, hh=hh: e.reduce_sum(ssg[:, hh * 2:hh * 2 + 2], scr[:, 0:D].rearrange("p (g d) -> p g d", g=2), axis=AX.X),
                            [b_scr], [b_ssg])
                    DVE(lambda e: e.tensor_scalar(ssg[:, 0:4], ssg[:, 0:4], 1.0 / 512, EPS, ALU.mult, ALU.add), [b_ssg], [b_ssg])
                    ACT(lambda e: e.activation(ssg[:, 4:8], ssg[:, 0:4], AF.Sqrt), [b_ssg], [b_ssg])
                    DVE(lambda e: e.reciprocal(ssg[:, 4:8], ssg[:, 4:8]), [b_ssg], [b_ssg])
                    DVE(lambda e: e.tensor_tensor(ynb[:].rearrange("p (g d) -> p g d", g=4), yk[:].rearrange("p (g d) -> p g d", g=4),
                                                  ssg[:, 4:8].unsqueeze(2).to_broadcast([128, 4, 512]), ALU.mult), [b_yk, b_ssg], [b_ynb])
                    for hh in range(2):
                        pt, bpt = tbank()
                        for q in range(8):
                            m = hh * 8 + q
                            PE(lambda e, m=m, q=q, pt=pt: e.transpose(pt[:, q * 128:(q + 1) * 128], ynb[:, m * 128:(m + 1) * 128], identb[:]),
                               [b_ynb, b_identb], [bpt])
                        for q in range(8):
                            m = hh * 8 + q
                            ACT(lambda e, pt=pt, m=m, q=q: e.activation(ynT[:, m, cs:cs + 128], pt[:, q * 128:(q + 1) * 128], AF.Identity,
                                                                        scale=ssdnw[:, m:m + 1]), [bpt, b_ssdnw], [b_ynT])

                sc_list = [("pre", i) for i in range(NSC)] + [("own", i) for i in range(NSC)]
                for (kind, si) in sc_list:
                    full = kind == "own"
                    xsrc = x_own if full else x_pre
                    last_pre = (kind == "pre" and si == NSC - 1)
                    for tt in range(4):
                        xt, bxt = xt4[tt % 2]
                        r0 = si * 512 + tt * 128
                        LD(xt[:], xsrc[r0:r0 + 128, :], bxt)
                        rms_to_T(xt[:], bxt, am_col, 0, hT, b_hT, tt * 128, scr, b_scr, stt, b_stt)
                    ck(2)
                    if full:
                        for zi in range(4):
                            pc, bpc = piece(w_in, 0, zi * 512, 512)
                            for tt in range(4):
                                ps, bps = fbank()
                                for k in range(8):
                                    PE(lambda e, k=k, tt=tt, pc=pc, ps=ps: e.matmul(ps[:, 0:512], hT[:, k, tt * 128:(tt + 1) * 128], pc[:, k, :],
                                                                                    start=(k == 0), stop=(k == 7)), [bpc, b_hT], [bps])
                                ACT(lambda e, tt=tt, zi=zi, ps=ps: e.activation(zs[:, tt, zi * 512:(zi + 1) * 512], ps[:, 0:512], AF.Silu), [bps], [b_zs])
                    for xi in range(4):
                        pc, bpc = piece(w_in, 0, 2048 + xi * 512, 512)
                        feat_major_tiles(pc, bpc, 4, xi * 4, conv_consumer)
                    pc, bpc = piece(w_in, 0, 4096, 512)
                    feat_major_tiles(pc, bpc, 4, 16, conv_consumer)
                    if full or last_pre:
                        pc, bpc = piece(w_in, 0, 4608, 512)
                        feat_major_tiles(pc, bpc, 4, 20, conv_consumer)
                    ck(3)
                    pc, bpc = piece(w_in, 0, 5120, 32)
                    for tt in range(4):
                        ps, bps = fbank()
                        for k in range(8):
                            PE(lambda e, k=k, tt=tt, pc=pc, ps=ps: e.matmul(ps[:, 0:32], hT[:, k, tt * 128:(tt + 1) * 128], pc[:, k, 0:32],
                                                                            start=(k == 0), stop=(k == 7)), [bpc, b_hT], [bps])
                        DVE(lambda e, tt=tt, ps=ps: e.tensor_tensor(dtt[:, tt, :], ps[:, 0:32], dtb_row[:], ALU.add), [bps, b_dtb], [b_dtt])
                    ACT(lambda e: e.activation(dtt[:], dtt[:], AF.Exp), [b_dtt], [b_dtt])
                    ACT(lambda e: e.activation(dtt[:], dtt[:], AF.Ln, bias=1.0), [b_dtt], [b_dtt])
                    DVE(lambda e: e.tensor_tensor(aa[:], dtt[:], A_row[:].unsqueeze(1).to_broadcast([128, 4, 32]), ALU.mult), [b_dtt, b_A], [b_aa])
                    ck(4)
                    if full or last_pre:
                        pcons = make_pool_consumer(full and si == 0)
                        for pi in range(2):
                            pc, bpc = piece(w_in, 0, 5152 + pi * 512, 512)
                            feat_major_tiles(pc, bpc, 4, pi * 4, pcons)
                    if full:
                        for gi in range(2):
                            pc, bpc = piece(w_in, 0, 7200 + gi * 512, 512)
                            feat_major_tiles(pc, bpc, 4, gi * 4,
                                             lambda m, ps, bps: ACT(lambda e: e.activation(gs[:, m, :], ps[:, 0:512], AF.Sigmoid), [bps], [b_gs]))
                        pc, bpc = piece(w_pool, 0, 0, 256)
                        for g in range(4):
                            for m2 in range(2):
                                ps, bps = fbank()
                                for k2 in range(2):
                                    PE(lambda e, g=g, m2=m2, k2=k2, pc=pc, ps=ps: e.matmul(ps[:, 0:512], pc[:, g * 2 + k2, m2 * 128:(m2 + 1) * 128],
                                                                                           pooledT[:, g * 2 + k2, :], start=(k2 == 0), stop=(k2 == 1)),
                                       [bpc, b_pooledT], [bps])
                                o_ = g * 2 + m2
                                ACT(lambda e, o_=o_, ps=ps: e.activation(pT[:, o_, :], ps[:, 0:512], AF.Identity, scale=pscale[:, o_:o_ + 1]),
                                    [bps, b_pscale], [b_pT])
                        for db in range(2):
                            pc, bpc = piece(w_pool_out, 0, db * 512, 512)
                            for mt in range(4):
                                d_ = db * 4 + mt
                                ps, bps = fbank()
                                for k in range(8):
                                    PE(lambda e, k=k, mt=mt, pc=pc, ps=ps: e.matmul(ps[:, 0:512], pc[:, k, mt * 128:(mt + 1) * 128], pT[:, k, :],
                                                                                    start=(k == 0), stop=(k == 7)), [bpc, b_pT], [bps])
                                DVE(lambda e, d_=d_, ps=ps: e.tensor_tensor(mergedT[:, d_, :], ps[:, 0:512], gs[:, d_, :], ALU.mult), [bps, b_gs], [b_mergedT])
                        ck(8)
                    ssd_prep()
                    for cq in range(4):
                        ssd_chunk(cq, full)
                        ck(5 if not full else 9)
                    if last_pre:
                        DVE(lambda e: e.tensor_scalar(hst[:], hst[:], flag[:, 0:1], None, ALU.mult), [b_hst, b_flag], [b_hst])
                        DVE(lambda e: e.tensor_scalar(hprev[:], hprev[:], flag[:, 0:1], None, ALU.mult), [b_hprev, b_flag], [b_hprev])
                        DVE(lambda e: e.tensor_scalar(halo[:].rearrange("p m j -> p (m j)"), halo[:].rearrange("p m j -> p (m j)"), flag[:, 0:1], None, ALU.mult),
                            [b_halo, b_flag], [b_halo])
                        DVE(lambda e: e.tensor_scalar(phalo[:].rearrange("p m j -> p (m j)"), phalo[:].rearrange("p m j -> p (m j)"), flag[:, 0:1], None, ALU.mult),
                            [b_phalo, b_flag], [b_phalo])
                    if last_pre:
                        ck(7)
                    if not full:
                        ck(6)
                        continue
                    for gi in range(2):
                        pc, bpc = piece(w_in, 0, 6176 + gi * 512, 512)
                        feat_major_tiles(pc, bpc, 4, gi * 4,
                                         lambda m, ps, bps: ACT(lambda e: e.activation(gs[:, m, :], ps[:, 0:512], AF.Sigmoid), [bps], [b_gs]))
                    for db in range(2):
                        pc0, bpc0 = piece(w_ssd_out, 0, db * 512, 512)
                        pc1, bpc1 = piece(w_ssd_out, 8, db * 512, 512)
                        for mt in range(4):
                            d_ = db * 4 + mt
                            ps, bps = fbank()
                            for k in range(16):
                                pcx, bpcx = (pc0, bpc0) if k < 8 else (pc1, bpc1)
                                PE(lambda e, k=k, mt=mt, pcx=pcx, ps=ps: e.matmul(ps[:, 0:512], pcx[:, k % 8, mt * 128:(mt + 1) * 128], ynT[:, k, :],
                                                                                  start=(k == 0), stop=(k == 15)), [bpcx, b_ynT], [bps])
                            DVE(lambda e, d_=d_, ps=ps: e.tensor_tensor(acc[:], ps[:, 0:512], gs[:, d_, :], ALU.mult), [bps, b_gs], [b_acc])
                            if os.environ.get('KDBG') != 'nossd':
                                DVE(lambda e, d_=d_: e.tensor_tensor(mergedT[:, d_, :], mergedT[:, d_, :], acc[:], ALU.add), [b_mergedT, b_acc], [b_mergedT])
                    pcs = [piece(w_out, 0, cb * 512, 512) for cb in range(2)]
                    for tt in range(4):
                        xt, bxt = xt4[tt % 2]
                        r0 = si * 512 + tt * 128
                        LD(xt[:], xsrc[r0:r0 + 128, :], bxt)
                        for cb in range(2):
                            pc, bpc = pcs[cb]
                            csl = slice(cb * 512, (cb + 1) * 512)
                            ps, bps = fbank()
                            for k in range(8):
                                PE(lambda e, k=k, tt=tt, pc=pc, ps=ps: e.matmul(ps[:, 0:512], mergedT[:, k, tt * 128:(tt + 1) * 128], pc[:, k, :],
                                                                                start=(k == 0), stop=(k == 7)), [bpc, b_mergedT], [bps])
                            DVE(lambda e, ps=ps, csl=csl: e.tensor_tensor(acc[:], ps[:, 0:512], garow[:, csl], ALU.mult), [bps, b_garow], [b_acc])
                            DVE(lambda e, xt=xt, csl=csl: e.tensor_tensor(xt[:, csl], acc[:], xt[:, csl], ALU.add), [b_acc, bxt], [bxt])
                        P.dma("sp", (x1_d[r0:r0 + 128, :], xt[:]), reads=[bxt], writes=[b_x1d])
                    ck(10)

            if n_exp < 0:
                return
            ck(11)
            with QuietStack() as es:
                def esb(name, shape, dt=F32):
                    t = es.enter_context(nc.sbuf_tensor("e%d_" % state["run"] + name, list(shape), dt))
                    return t, Buf(name)

                x1, b_x1 = esb("x1", [128, 16, D])
                h2T, b_h2T = esb("h2T", [128, 8, T_CORE], BF16)
                actT32, b_actT = esb("actT32", [128, 8, T_CORE // 2])
                actT = actT32[:].bitcast(BF16)
                h32, b_h32 = actT32[:, :, 0:128], b_actT
                scr2, b_scr2 = esb("scr2", [128, D])
                st2, b_st2 = esb("st2", [128, 8])
                wr, b_wr = esb("wr", [128, 8, 32])
                brr, b_brr = esb("brr", [128, 32])
                gate, b_gate = esb("gate", [128, 16, 32])
                gateT, b_gateT = esb("gateT", [128, T_CORE], BF16)
                gpad, b_gpad = esb("gpad", [128, 128])
                lg, b_lg = esb("lg", [128, 32])
                top8, b_top8 = esb("top8", [128, 8])
                msk, b_msk = esb("msk", [128, 32])
                bgu, b_bgu = esb("bgu", [128, 32, 16])
                bdn, b_bdn = esb("bdn", [128, D], BF16)
                glu, b_glu = esb("glu", [128, 512])
                sig, b_sig = esb("sig", [128, 512])
                lin, b_lin = esb("lin", [128, 512])
                ev, b_ev = esb("ev", [128, 512])

                LD(wr[:], w_router_d.rearrange("(k p) e -> p k e", p=128), b_wr)
                LD(brr[:], brouter_row_d, b_brr)
                LD(bgu[:], bgu_col_d, b_bgu)
                LD(scr2[:], b_down_d, b_scr2)
                DVE(lambda e: e.tensor_copy(bdn[:], scr2[:]), [b_scr2], [b_bdn])
                DVE(lambda e: e.memset(gpad[:], 0.0), [], [b_gpad])
                for tt in range(16):
                    P.dma("sp", (x1[:, tt, :], x1_d[tt * 128:(tt + 1) * 128, :]), reads=[b_x1d], writes=[b_x1])
                for tt in range(16):
                    rms_to_T(x1[:, tt, :], b_x1, af_col, 24, h2T, b_h2T, tt * 128, scr2, b_scr2, st2, b_st2, h32=h32, bh32=b_h32)
                    ps, bps = fbank()
                    for k in range(8):
                        PE(lambda e, k=k, ps=ps: e.matmul(ps[:, 0:32], h32[:, k, :], wr[:, k, :], start=(k == 0), stop=(k == 7)), [b_h32, b_wr], [bps])
                    DVE(lambda e, ps=ps: e.tensor_tensor(lg[:], ps[:, 0:32], brr[:], ALU.add), [bps, b_brr], [b_lg])
                    DVE(lambda e: e.max(top8[:], lg[:]), [b_lg], [b_top8])
                    DVE(lambda e: e.tensor_scalar(msk[:], lg[:], top8[:, 3:4], None, ALU.is_ge), [b_lg, b_top8], [b_msk])
                    DVE(lambda e: e.tensor_scalar(lg[:], lg[:], top8[:, 0:1], None, ALU.subtract), [b_lg, b_top8], [b_lg])
                    ACT(lambda e: e.activation(lg[:], lg[:], AF.Exp), [b_lg], [b_lg])
                    DVE(lambda e: e.tensor_tensor(lg[:], lg[:], msk[:], ALU.mult), [b_lg, b_msk], [b_lg])
                    DVE(lambda e: e.reduce_sum(st2[:, 4:5], lg[:], axis=AX.X), [b_lg], [b_st2])
                    DVE(lambda e: e.reciprocal(st2[:, 5:6], st2[:, 4:5]), [b_st2], [b_st2])
                    DVE(lambda e, tt=tt: e.tensor_scalar(gate[:, tt, :], lg[:], st2[:, 5:6], None, ALU.mult), [b_lg, b_st2], [b_gate])
                    ps, bps = fbank()
                    DVE(lambda e, tt=tt: e.tensor_copy(gpad[:, 0:32], gate[:, tt, :]), [b_gate], [b_gpad])
                    PE(lambda e, ps=ps: e.transpose(ps[:, 0:128], gpad[:], ident[:]), [b_gpad, b_ident], [bps])
                    ACT(lambda e, tt=tt, ps=ps: e.activation(gateT[:, tt * 128:(tt + 1) * 128], ps[:, 0:128], AF.Identity), [bps], [b_gateT])
                ck(12)
                for tt in range(16):
                    for cb in range(2):
                        csl = slice(cb * 512, (cb + 1) * 512)
                        ps, bps = fbank()
                        PE(lambda e, tt=tt, csl=csl, ps=ps: e.matmul(ps[:, 0:512], gateT[:, tt * 128:(tt + 1) * 128], bdn[:, csl], start=True, stop=True),
                           [b_gateT, b_bdn], [bps])
                        DVE(lambda e, ps=ps, cb=cb: e.tensor_tensor(ev[:], ps[:, 0:512], garow[:, 1024 + cb * 512:1024 + (cb + 1) * 512], ALU.mult),
                            [bps, b_garow], [b_ev])
                        DVE(lambda e, tt=tt, csl=csl: e.tensor_tensor(x1[:, tt, csl], x1[:, tt, csl], ev[:], ALU.add), [b_x1, b_ev], [b_x1])
                for ex in range(n_exp):
                    for j in range(2):
                        pg_, bpg_ = piece(w_gu[ex], 0, j * 512, 512)
                        pl_, bpl_ = piece(w_gu[ex], 0, 1024 + j * 512, 512)
                        for tc in range(4):
                            tsl = slice(tc * 512, (tc + 1) * 512)
                            for mt in range(4):
                                m = j * 4 + mt
                                p1, bp1 = fbank()
                                for k in range(8):
                                    PE(lambda e, k=k, mt=mt, p1=p1, pg_=pg_, tsl=tsl: e.matmul(p1[:, 0:512], pg_[:, k, mt * 128:(mt + 1) * 128], h2T[:, k, tsl],
                                                                                               start=(k == 0), stop=(k == 7)), [bpg_, b_h2T], [bp1])
                                p2, bp2 = fbank()
                                for k in range(8):
                                    PE(lambda e, k=k, mt=mt, p2=p2, pl_=pl_, tsl=tsl: e.matmul(p2[:, 0:512], pl_[:, k, mt * 128:(mt + 1) * 128], h2T[:, k, tsl],
                                                                                               start=(k == 0), stop=(k == 7)), [bpl_, b_h2T], [bp2])
                                DVE(lambda e, p1=p1, m=m, ex=ex: e.tensor_scalar(glu[:], p1[:, 0:512], bgu[:, ex, m:m + 1], 7.0, ALU.add, ALU.min),
                                    [bp1, b_bgu], [b_glu])
                                ACT(lambda e: e.activation(sig[:], glu[:], AF.Sigmoid, scale=1.702), [b_glu], [b_sig])
                                DVE(lambda e, p2=p2, m=m, ex=ex: e.tensor_scalar(lin[:], p2[:, 0:512], bgu[:, ex, 8 + m:8 + m + 1], 7.0, ALU.add, ALU.min),
                                    [bp2, b_bgu], [b_lin])
                                DVE(lambda e: e.tensor_scalar(lin[:], lin[:], -7.0, 1.0, ALU.max, ALU.add), [b_lin], [b_lin])
                                DVE(lambda e: e.tensor_tensor(glu[:], glu[:], sig[:], ALU.mult), [b_glu, b_sig], [b_glu])
                                DVE(lambda e, m=m, tsl=tsl: e.tensor_tensor(actT[:, m, tsl], glu[:], lin[:], ALU.mult), [b_glu, b_lin], [b_actT])
                    pds = [piece(w_down[ex], 0, cb * 512, 512) for cb in range(2)]
                    for tt in range(16):
                        for cb in range(2):
                            pd_, bpd_ = pds[cb]
                            csl = slice(cb * 512, (cb + 1) * 512)
                            ps, bps = fbank()
                            for k in range(8):
                                PE(lambda e, k=k, tt=tt, ps=ps, pd_=pd_: e.matmul(ps[:, 0:512], actT[:, k, tt * 128:(tt + 1) * 128], pd_[:, k, :],
                                                                                  start=(k == 0), stop=(k == 7)), [bpd_, b_actT], [bps])
                            DVE(lambda e, ps=ps, cb=cb: e.tensor_tensor(ev[:], ps[:, 0:512], garow[:, 1024 + cb * 512:1024 + (cb + 1) * 512], ALU.mult),
                                [bps, b_garow], [b_ev])
                            DVE(lambda e, tt=tt, csl=csl, ex=ex: e.scalar_tensor_tensor(x1[:, tt, csl], ev[:], gate[:, tt, ex:ex + 1], x1[:, tt, csl],
                                                                                        ALU.mult, ALU.add), [b_ev, b_gate, b_x1], [b_x1])
                nfin, b_nfin = actT32[:, 0, :], b_actT
                LD(nfin, nfin_row_d, b_nfin)
                ck(13)
                for tt in range(16):
                    ACT(lambda e, tt=tt: e.activation(scr2[:], x1[:, tt, :], AF.Square), [b_x1], [b_scr2])
                    DVE(lambda e: e.reduce_sum(st2[:, 0:1], scr2[:], axis=AX.X), [b_scr2], [b_st2])
                    DVE(lambda e: e.tensor_scalar(st2[:, 1:2], st2[:, 0:1], 1.0 / D, EPS, ALU.mult, ALU.add), [b_st2], [b_st2])
                    ACT(lambda e: e.activation(st2[:, 3:4], st2[:, 1:2], AF.Sqrt), [b_st2], [b_st2])
                    DVE(lambda e: e.reciprocal(st2[:, 2:3], st2[:, 3:4]), [b_st2], [b_st2])
                    DVE(lambda e, tt=tt: e.scalar_tensor_tensor(scr2[:], x1[:, tt, :], st2[:, 2:3], nfin, ALU.mult, ALU.mult),
                        [b_x1, b_st2, b_nfin], [b_scr2])
                    P.dma("sp", (out_d[tt * 128:(tt + 1) * 128, :], scr2[:]), reads=[b_scr2], is_output=True)

        b_x1d = Buf("x1d")
        Pd = Prog(nc, st, dry=True)
        try:
            program(Pd)
        except _Stop:
            pass
        P = Prog(nc, st)
        try:
            program(P)
        except _Stop:
            pass
        with nc.Block() as block:
            P.emit(block)
        _LAST['seq'] = dict(P.seq)
    return nc


def _col(v, n):
    return np.ascontiguousarray(np.asarray(v, np.float32).reshape(n, 128).T)


def _row(v):
    v = np.asarray(v, np.float32).reshape(1, -1)
    return np.ascontiguousarray(np.broadcast_to(v, (128, v.shape[1])))


def make_in_maps(x, c, w_ada, b_ada, norm_mix_w, w_in, conv_w, conv_b, dt_bias, a_log, d_skip,
                 ssd_norm_w, w_ssd_out, w_pool, pool_scale, w_pool_out, w_out, norm_ffn_w,
                 w_router, b_router, w_gu, b_gu, w_down, b_down, norm_final_w):
    f = lambda a: np.ascontiguousarray(np.asarray(a, np.float32))
    x = f(x)
    ident = np.eye(128, dtype=np.float32)
    utri = np.triu(np.ones((128, 128), np.float32))
    negm = np.where(np.arange(128)[None, :] < np.arange(128)[:, None], NEG, 0.0).astype(np.float32)
    negm4 = np.ascontiguousarray(np.tile(negm, (1, 4)))
    shared = dict(
        w_ada=f(w_ada[0]), bada_col=_col(b_ada[0], 48),
        bada_row=np.ascontiguousarray(np.concatenate([_row(b_ada[0][2048:3072]), _row(b_ada[0][5120:6144])], axis=1)),
        nmw_col=_col(norm_mix_w[0], 8), nfw_col=_col(norm_ffn_w[0], 8), nfin_row=_row(norm_final_w),
        w_in=f(w_in[0]),
        convw_col=np.ascontiguousarray(np.asarray(conv_w[0], np.float32).reshape(4, 24, 128).transpose(2, 1, 0)),
        convb_col=_col(conv_b[0], 24),
        dtb_row=_row(dt_bias[0]), alog_row=_row(a_log[0]), dskip_row=_row(d_skip[0]),
        ssdnw_col=_col(ssd_norm_w[0], 16), w_ssd_out=f(w_ssd_out[0]),
        w_pool=f(np.asarray(w_pool[0]).reshape(1024, 256)), pscale_col=_col(pool_scale[0], 8),
        w_pool_out=f(w_pool_out[0]), w_out=f(w_out[0]),
        w_router=f(w_router[0]), brouter_row=_row(b_router[0]),
        w_gu=f(w_gu[0]), bgu_col=np.ascontiguousarray(np.asarray(b_gu[0], np.float32).reshape(32, 16, 128).transpose(2, 0, 1)),
        w_down=f(w_down[0]), b_down=np.ascontiguousarray(np.concatenate([f(b_down[0]), np.zeros((96, 1024), np.float32)], 0)),
        ident=ident, utri=utri, negm4=negm4,
    )
    maps = []
    for core in range(8):
        b, hf = core // 2, core % 2
        inv = np.empty((4, 512), np.float32)
        t = np.arange(512)
        for wi, w in enumerate((2, 4, 8, 16)):
            inv[wi] = 1.0 / (np.minimum(t + 1, w) if hf == 0 else w)
        m = dict(shared)
        m["x_own"] = np.ascontiguousarray(x[b, hf * 2048:(hf + 1) * 2048])
        m["x_pre"] = np.ascontiguousarray(x[b, 0:2048])
        m["flag"] = np.full((128, 1), float(hf), np.float32)
        m["invcnt"] = np.ascontiguousarray(np.broadcast_to(inv[None], (128, 4, 512)))
        m["c_col"] = _col(np.asarray(c, np.float32)[b], 8)
        maps.append(m)
    return maps


_NC_CACHE = {}


def kernel(**inputs):
    if "nc" not in _NC_CACHE:
        _NC_CACHE["nc"] = build_nc()
    nc = _NC_CACHE["nc"]
    maps = make_in_maps(**inputs)
    res = run_bass_kernel_spmd(nc, maps, core_ids=list(range(8)))
    out = np.empty((4, 4096, 1024), np.float32)
    for core in range(8):
        b, hf = core // 2, core % 2
        out[b, hf * 2048:(hf + 1) * 2048] = res.results[core]["out"]
    return out
```
